# Optimizing a Trainium2 kernel written in Bass

```python
import math
import jax, jax.numpy as jnp
from jax import lax
import numpy as np


D_MODEL = 2048
BATCH = 4
SEQ = 4096
DEPTH = 4

N_MIXERS = 3
EPS = 1e-6

M_HEADS = 4
M_DQK = 256
M_DV = 512
M_CHUNK = 64
M_GATE_CAP = 15.0
M_COLS = M_HEADS * (2 * M_DQK + 2 * M_DV) + 2 * M_HEADS

A_HEADS = 32
A_KV_HEADS = 4
A_HEAD_DIM = 64
WINDOW = 128
A_COLS = (A_HEADS + 2 * A_KV_HEADS) * A_HEAD_DIM

R_HEADS = 8
R_DQK = 256
R_DV = 512
R_CHUNK = 128
R_COLS = R_HEADS * (2 * R_DQK + 2 * R_DV)

D_FF = 5632
CONV_WIDTH = 3

N_A = (DEPTH + 2) // 3
N_B = (DEPTH + 1) // 3
N_C = DEPTH // 3

kernel_name = 'hybrid_mlstm_swa_retention_convffn'


def rms_norm(x, gain):
    xf = x.astype(jnp.float32)
    y = xf * lax.rsqrt(jnp.mean(xf * xf, axis=-1, keepdims=True) + EPS)
    return (y * gain).astype(x.dtype)


def mlstm_mixer(x, w_in, gate_b, head_norm, w_out):
    f32 = jnp.float32
    bsz, seq, _ = x.shape
    H, dk, dv, L = M_HEADS, M_DQK, M_DV, M_CHUNK
    nc = seq // L
    proj = x @ w_in
    q, k, v, o, ig, fg = jnp.split(
        proj, [H * dk, 2 * H * dk, 2 * H * dk + H * dv, 2 * H * (dk + dv), 2 * H * (dk + dv) + H], axis=-1)
    ig = M_GATE_CAP * jnp.tanh((ig + gate_b[0]).astype(f32) / M_GATE_CAP)
    fg = M_GATE_CAP * jnp.tanh((fg + gate_b[1]).astype(f32) / M_GATE_CAP)
    logf = jax.nn.log_sigmoid(fg)

    def heads(t, d):
        return t.astype(f32).reshape(bsz, nc, L, H, d).transpose(1, 0, 3, 2, 4)

    def gates(t):
        return t.reshape(bsz, nc, L, H).transpose(1, 0, 3, 2)

    qc = heads(q, dk) * dk ** -0.5
    kc = heads(k, dk)
    vc = heads(v, dv)
    igc = gates(ig)
    lfc = gates(logf)
    causal = jnp.tril(jnp.ones((L, L), dtype=bool))

    def step(carry, inp):
        C, n, m = carry
        qb, kb, vb, ib, fb = inp
        b = jnp.cumsum(fb, axis=-1)
        dmat = jnp.where(causal, b[..., :, None] - b[..., None, :] + ib[..., None, :], -jnp.inf)
        inter = b + m[..., None]
        m_t = jnp.maximum(inter, jnp.max(dmat, axis=-1))
        w_intra = jnp.exp(dmat - m_t[..., None])
        w_inter = jnp.exp(inter - m_t)
        s = jnp.einsum('bhtd,bhsd->bhts', qb, kb) * w_intra
        num = jnp.einsum('bhts,bhsv->bhtv', s, vb) + w_inter[..., None] * jnp.einsum('bhtd,bhvd->bhtv', qb, C)
        den = jnp.sum(s, axis=-1) + w_inter * jnp.einsum('bhtd,bhd->bht', qb, n)
        h = num / jnp.maximum(jnp.abs(den), jnp.exp(-m_t))[..., None]
        g = b[..., -1]
        a = g[..., None] - b + ib
        m_new = jnp.maximum(g + m, jnp.max(a, axis=-1))
        keep = jnp.exp(g + m - m_new)
        wk = jnp.exp(a - m_new[..., None])
        C_new = keep[..., None, None] * C + jnp.einsum('bhs,bhsv,bhsd->bhvd', wk, vb, kb)
        n_new = keep[..., None] * n + jnp.einsum('bhs,bhsd->bhd', wk, kb)
        return (C_new, n_new, m_new), h

    init = (jnp.zeros((bsz, H, dv, dk), f32), jnp.zeros((bsz, H, dk), f32), jnp.zeros((bsz, H), f32))
    _, hc = lax.scan(step, init, (qc, kc, vc, igc, lfc))
    h = hc.transpose(1, 0, 3, 2, 4).reshape(bsz, seq, H, dv)
    h = h * lax.rsqrt(jnp.mean(h * h, axis=-1, keepdims=True) + EPS) * head_norm.reshape(H, dv).astype(f32)
    h = h.reshape(bsz, seq, H * dv) * jax.nn.sigmoid(o.astype(f32))
    return h.astype(x.dtype) @ w_out


def swa_sink_mixer(x, w_qkv, sinks, w_out):
    f32 = jnp.float32
    bsz, seq, _ = x.shape
    H, KV, hd, W = A_HEADS, A_KV_HEADS, A_HEAD_DIM, WINDOW
    G = H // KV
    nb = seq // W
    qkv = x @ w_qkv
    q, k, v = jnp.split(qkv, [H * hd, (H + KV) * hd], axis=-1)
    q = q.reshape(bsz, nb, W, KV, G, hd) * hd ** -0.5
    k = k.reshape(bsz, nb, W, KV, hd)
    v = v.reshape(bsz, nb, W, KV, hd)

    def with_prev(t):
        prev = jnp.pad(t[:, :-1], ((0, 0), (1, 0), (0, 0), (0, 0), (0, 0)))
        return jnp.concatenate([prev, t], axis=2)

    kb, vb = with_prev(k), with_prev(v)
    scores = jnp.einsum('bnqkgd,bnskd->bnkgqs', q, kb).astype(f32)
    qi = jnp.arange(W)[:, None]
    kj = jnp.arange(2 * W)[None, :]
    rel = qi + W - kj
    band = (rel >= 0) & (rel < W)
    key_abs = jnp.arange(nb)[:, None, None] * W + kj[None] - W
    valid = band[None] & (key_abs >= 0)
    scores = jnp.where(valid[None, :, None, None], scores, -jnp.inf)
    sink = sinks.astype(f32).reshape(KV, G)[None, None, :, :, None]
    mx = jnp.maximum(jnp.max(scores, axis=-1), sink)
    p = jnp.exp(scores - mx[..., None])
    denom = jnp.sum(p, axis=-1) + jnp.exp(sink - mx)
    p = p / denom[..., None]
    out = jnp.einsum('bnkgqs,bnskd->bnqkgd', p.astype(v.dtype), vb).reshape(bsz, seq, H * hd)
    return out @ w_out


def rotate(t, pos):
    half = t.shape[-1] // 2
    inv = jnp.power(10000.0, -jnp.arange(half, dtype=jnp.float32) / half)
    ang = pos[:, None] * inv[None, :]
    cos = jnp.cos(ang)[None, :, None, :]
    sin = jnp.sin(ang)[None, :, None, :]
    t1, t2 = t[..., :half], t[..., half:]
    return jnp.concatenate([t1 * cos - t2 * sin, t1 * sin + t2 * cos], axis=-1)


def retention_mixer(x, w_in, head_norm, w_out):
    f32 = jnp.float32
    bsz, seq, _ = x.shape
    H, dk, dv, L = R_HEADS, R_DQK, R_DV, R_CHUNK
    nc = seq // L
    proj = x @ w_in
    q, k, v, g = jnp.split(proj, [H * dk, 2 * H * dk, 2 * H * dk + H * dv], axis=-1)
    pos = jnp.arange(seq, dtype=f32)
    q = rotate(q.astype(f32).reshape(bsz, seq, H, dk), pos)
    k = rotate(k.astype(f32).reshape(bsz, seq, H, dk), pos) * dk ** -0.5
    v = v.astype(f32).reshape(bsz, seq, H, dv)

    def chunks(t, d):
        return t.reshape(bsz, nc, L, H, d).transpose(1, 0, 3, 2, 4)

    qc, kc, vc = chunks(q, dk), chunks(k, dk), chunks(v, dv)
    lg = jnp.log(1.0 - jnp.power(2.0, -5.0 - jnp.arange(H, dtype=f32)))
    lp = jnp.arange(L, dtype=f32)
    rel = lp[:, None] - lp[None, :]
    dmask = jnp.where(rel >= 0, jnp.exp(lg[:, None, None] * jnp.maximum(rel, 0.0)), 0.0)
    inter_decay = jnp.exp(lg[:, None] * (lp + 1.0))
    state_w = jnp.exp(lg[:, None] * (L - 1.0 - lp))
    chunk_decay = jnp.exp(lg * L)

    def step(R, inp):
        qb, kb, vb = inp
        s = jnp.einsum('bhtd,bhsd->bhts', qb, kb) * dmask
        out = jnp.einsum('bhts,bhsv->bhtv', s, vb) + jnp.einsum('bhtd,bhdv->bhtv', qb, R) * inter_decay[..., None]
        R_new = chunk_decay[:, None, None] * R + jnp.einsum('bhsd,hs,bhsv->bhdv', kb, state_w, vb)
        return R_new, out

    _, oc = lax.scan(step, jnp.zeros((bsz, H, dk, dv), f32), (qc, kc, vc))
    o = oc.transpose(1, 0, 3, 2, 4).reshape(bsz, seq, H, dv)
    mu = jnp.mean(o, axis=-1, keepdims=True)
    var = jnp.mean(jnp.square(o - mu), axis=-1, keepdims=True)
    o = (o - mu) * lax.rsqrt(var + EPS) * head_norm.reshape(H, dv).astype(f32)
    o = o.reshape(bsz, seq, H * dv) * jax.nn.silu(g.astype(f32))
    return o.astype(x.dtype) @ w_out


def conv_ffn(x, w_up, conv_w, conv_b, w_down):
    seq = x.shape[1]
    u = x @ w_up
    up = jnp.pad(u, ((0, 0), (CONV_WIDTH - 1, 0), (0, 0)))
    c = conv_b + sum(conv_w[j] * up[:, j:j + seq] for j in range(CONV_WIDTH))
    gate, val = jnp.split(c, 2, axis=-1)
    return (jax.nn.silu(gate) * val) @ w_down


def setup_inputs(seed: int = 0) -> dict:
    key = jax.random.key(seed)
    ks = jax.random.split(key, 20)
    f32 = jnp.float32

    def dense(k, shape, fan_in):
        return jax.random.normal(k, shape, f32) * fan_in ** -0.5

    def gain(k, shape):
        return 1.0 + 0.02 * jax.random.normal(k, shape, f32)

    x = jax.random.normal(ks[0], (BATCH, SEQ, D_MODEL), f32)
    norm_gains = gain(ks[1], (DEPTH, 4, D_MODEL))
    ffn_w_up = dense(ks[2], (DEPTH, D_MODEL, 2 * D_FF), D_MODEL)
    ffn_conv_w = dense(ks[3], (DEPTH, CONV_WIDTH, 2 * D_FF), CONV_WIDTH)
    ffn_conv_b = 0.02 * jax.random.normal(ks[4], (DEPTH, 2 * D_FF), f32)
    ffn_w_down = dense(ks[5], (DEPTH, D_FF, D_MODEL), D_FF)
    mlstm_w_in = dense(ks[6], (N_A, D_MODEL, M_COLS), D_MODEL)
    ig_b = 0.1 * jax.random.normal(ks[7], (N_A, M_HEADS), f32)
    fg_b = jnp.linspace(3.0, 6.0, M_HEADS, dtype=f32)[None] + 0.1 * jax.random.normal(ks[8], (N_A, M_HEADS), f32)
    mlstm_gate_b = jnp.stack([ig_b, fg_b], axis=1)
    mlstm_head_norm = gain(ks[9], (N_A, M_HEADS * M_DV))
    mlstm_w_out = dense(ks[10], (N_A, M_HEADS * M_DV, D_MODEL), M_HEADS * M_DV)
    swa_w_qkv = dense(ks[11], (N_B, D_MODEL, A_COLS), D_MODEL)
    swa_sinks = 0.5 * jax.random.normal(ks[12], (N_B, A_HEADS), f32)
    swa_w_out = dense(ks[13], (N_B, A_HEADS * A_HEAD_DIM, D_MODEL), A_HEADS * A_HEAD_DIM)
    ret_w_in = dense(ks[14], (N_C, D_MODEL, R_COLS), D_MODEL)
    ret_head_norm = gain(ks[15], (N_C, R_HEADS * R_DV))
    ret_w_out = dense(ks[16], (N_C, R_HEADS * R_DV, D_MODEL), R_HEADS * R_DV)
    return {'x': x, 'norm_gains': norm_gains, 'ffn_w_up': ffn_w_up, 'ffn_conv_w': ffn_conv_w,
            'ffn_conv_b': ffn_conv_b, 'ffn_w_down': ffn_w_down, 'mlstm_w_in': mlstm_w_in,
            'mlstm_gate_b': mlstm_gate_b, 'mlstm_head_norm': mlstm_head_norm, 'mlstm_w_out': mlstm_w_out,
            'swa_w_qkv': swa_w_qkv, 'swa_sinks': swa_sinks, 'swa_w_out': swa_w_out,
            'ret_w_in': ret_w_in, 'ret_head_norm': ret_head_norm, 'ret_w_out': ret_w_out}


def reference(x, norm_gains, ffn_w_up, ffn_conv_w, ffn_conv_b, ffn_w_down, mlstm_w_in, mlstm_gate_b,
              mlstm_head_norm, mlstm_w_out, swa_w_qkv, swa_sinks, swa_w_out, ret_w_in, ret_head_norm, ret_w_out):
    h = x
    for i in range(DEPTH):
        kind, j = i % N_MIXERS, i // N_MIXERS
        a = rms_norm(h, norm_gains[i, 0])
        if kind == 0:
            a = mlstm_mixer(a, mlstm_w_in[j], mlstm_gate_b[j], mlstm_head_norm[j], mlstm_w_out[j])
        elif kind == 1:
            a = swa_sink_mixer(a, swa_w_qkv[j], swa_sinks[j], swa_w_out[j])
        else:
            a = retention_mixer(a, ret_w_in[j], ret_head_norm[j], ret_w_out[j])
        h = h + rms_norm(a, norm_gains[i, 1])
        f = conv_ffn(rms_norm(h, norm_gains[i, 2]), ffn_w_up[i], ffn_conv_w[i], ffn_conv_b[i], ffn_w_down[i])
        h = h + rms_norm(f, norm_gains[i, 3])
    return h
```

```python
import math
from contextlib import ExitStack
import numpy as np
import concourse.bass as bass
import concourse.mybir as mybir
from concourse.bass_utils import run_bass_kernel_spmd

F32 = mybir.dt.float32
BF16 = mybir.dt.bfloat16
AF = mybir.ActivationFunctionType
ALU = mybir.AluOpType
AX = mybir.AxisListType

D = 2048
DFF = 5632
NFT = DFF // 128
EPS = 1e-6
NSD = 12


class Buf:
    __slots__ = ("name", "w", "r")

    def __init__(self, name):
        self.name = name
        self.w = None
        self.r = []


class Sched:
    ENG = ["pe", "act", "dve", "pool", "sp"]

    def __init__(self, nc, es):
        self.nc = nc
        self.h = {"pe": nc.tensor, "act": nc.scalar, "dve": nc.vector, "pool": nc.gpsimd, "sp": nc.sync}
        self.sem = {e: es.enter_context(nc.semaphore("c_" + e)) for e in self.ENG}
        self.cnt = {e: 0 for e in self.ENG}
        self.dsem = {e: [es.enter_context(nc.semaphore(f"d_{e}{i}")) for i in range(NSD)] for e in ("sp", "pool")}
        self.ndma = {"sp": 0, "pool": 0}
        self.wc = {e: {} for e in self.ENG}
        self.wd = {e: set() for e in self.ENG}

    def _wait(self, eng, tok):
        kind, e2, v = tok
        E = self.h[eng]
        if kind == "c":
            if e2 == eng and eng == "pe":
                return
            if self.wc[eng].get(e2, 0) >= v:
                return
            self.wc[eng][e2] = v
            E.wait_ge(self.sem[e2], v)
        else:
            if tok in self.wd[eng]:
                return
            self.wd[eng].add(tok)
            E.wait_ge(self.dsem[e2][v % NSD], 16 * (v // NSD + 1))

    def _deps(self, eng, reads, writes):
        deps = []
        for b in reads:
            if b.w is not None:
                deps.append(b.w)
        for b in writes:
            if b.w is not None:
                deps.append(b.w)
            deps.extend(b.r)
        best = {}
        for d in deps:
            if d[0] == "c":
                if best.get(d[1], 0) < d[2]:
                    best[d[1]] = d[2]
            else:
                self._wait(eng, d)
        for e2, v in best.items():
            self._wait(eng, ("c", e2, v))

    def _mark(self, tok, reads, writes):
        for b in reads:
            b.r.append(tok)
        for b in writes:
            b.w = tok
            b.r = []

    def op(self, eng, fn, reads=(), writes=()):
        self._deps(eng, reads, writes)
        ins = fn()
        self.cnt[eng] += 1
        ins.then_inc(self.sem[eng], 1)
        tok = ("c", eng, self.cnt[eng])
        self._mark(tok, reads, writes)
        return tok

    def dma(self, eng, out, in_, reads=(), writes=()):
        k = self.ndma[eng]
        self.ndma[eng] += 1
        if k >= NSD:
            self._wait(eng, ("d", eng, k - NSD))
        self._deps(eng, reads, writes)
        ins = self.h[eng].dma_start(out=out, in_=in_)
        ins.then_inc(self.dsem[eng][k % NSD], 16)
        tok = ("d", eng, k)
        self._mark(tok, reads, writes)
        return tok

    def barrier(self):
        toks = []
        for e in self.ENG:
            if self.cnt[e] > 0:
                toks.append(("c", e, self.cnt[e]))
        for e in ("sp", "pool"):
            for k in range(max(0, self.ndma[e] - NSD), self.ndma[e]):
                toks.append(("d", e, k))
        for e in self.ENG:
            for t in toks:
                if t[0] == "c" and t[1] == e:
                    if self.wc[e].get(e, 0) < t[2]:
                        self.wc[e][e] = t[2]
                        self.h[e].wait_ge(self.sem[e], t[2])
                    continue
                self._wait(e, t)


class Builder:
    def __init__(self, T, TS, layers):
        self.T, self.TS, self.layers = T, TS, layers
        self.NT = TS // 128
        self.NB = TS // 512
        self.NST = T // TS
        self.BN = 256
        self.nc = bass.Bass("TRN2", target_bir_lowering=False)
        self.es = ExitStack()
        self.evk = 0
        self.stg = 0

    def dram(self, name, shape, dt=F32, kind="ExternalInput"):
        return self.nc.dram_tensor(name, list(shape), dt, kind=kind).ap()

    def psl(self, es, name, dt, w):
        self.uid = getattr(self, "uid", 0) + 1
        return es.enter_context(self.nc.psum_tensor(f"{name}_{self.uid}", [128, w], dt))

    def sb(self, es, name, shape, dt):
        self.uid = getattr(self, "uid", 0) + 1
        return es.enter_context(self.nc.sbuf_tensor(f"{name}_{self.uid}", list(shape), dt))

    def evac_copy(self, out, in_, reads, writes):
        self.evk += 1
        if self.evk % 2 == 0:
            self.S.op("act", lambda: self.nc.scalar.activation(out=out, in_=in_, func=AF.Copy), reads, writes)
        else:
            self.S.op("dve", lambda: self.nc.vector.tensor_copy(out=out, in_=in_), reads, writes)

    def load_w(self, slot, W, row0, KT, segs):
        wt = self.wslots[slot]
        off = 0
        for i, (c0, n) in enumerate(segs):
            src = W[row0:row0 + KT * 128, c0:c0 + n].rearrange("(kt p) c -> p kt c", p=128)
            self.S.dma("pool", wt[:, 0:KT, off:off + n], src, reads=(), writes=(self.wbuf[slot][i],))
            off += n
        return off

    def get_w(self, W, row0, KT, segs):
        key = (id(W.tensor) if hasattr(W, "tensor") else 0, str(W), row0, KT, tuple(segs))
        if self.pref is not None and self.pref[0] == key:
            slot, ncols = self.pref[1], self.pref[2]
            self.pref = None
            return slot, ncols
        assert self.pref is None, "unused prefetch"
        slot = self.wnext % 2
        self.wnext += 1
        return slot, self.load_w(slot, W, row0, KT, segs)

    def prefetch_w(self, W, row0, KT, segs):
        key = (id(W.tensor) if hasattr(W, "tensor") else 0, str(W), row0, KT, tuple(segs))
        slot = self.wnext % 2
        self.wnext += 1
        self.pref = (key, slot, self.load_w(slot, W, row0, KT, segs))

    def lin_feat(self, W, row0, KT, groups, rhs_ap, rhs_bufs, evac, block_outer=False, ncolmax=512):
        nc, S = self.nc, self.S
        for gi, segs in enumerate(groups):
            slot, ncols = self.get_w(W, row0, KT, segs)
            wt = self.wslots[slot]
            nm = ncols // 128

            def group(mi, b, bank):
                pa, pb = self.lbank[bank]

                def fn():
                    ins = None
                    for kt in range(KT):
                        ins = nc.tensor.matmul(pa[:, 0:512], lhsT=wt[:, kt, mi * 128:(mi + 1) * 128],
                                               rhs=rhs_ap(kt, b), start=(kt == 0), stop=(kt == KT - 1))
                    return ins
                S.op("pe", fn, reads=self.wbuf[slot] + rhs_bufs(b), writes=[pb])
                return pa, pb

            if block_outer:
                for b in range(self.NB):
                    outs = []
                    for mi in range(nm):
                        outs.append(group(mi, b, mi % 4))
                    evac(gi, b, outs)
            else:
                for mi in range(nm):
                    for b in range(self.NB):
                        bank = self.lnext % 4
                        self.lnext += 1
                        pa, pb = group(mi, b, bank)
                        evac(gi, mi, b, pa, pb)

    def lin_tok(self, W, row0, KT, segs, evac):
        nc, S = self.nc, self.S
        slot, ncols = self.get_w(W, row0, KT, segs)
        wt = self.wslots[slot]
        for ti in range(self.NT):
            bank = self.lnext % 4
            self.lnext += 1
            pa, pb = self.lbank[bank]

            def fn():
                ins = None
                for kt in range(KT):
                    ins = nc.tensor.matmul(pa[:, 0:ncols], lhsT=self.rhs3[:, kt, ti * 128:(ti + 1) * 128],
                                           rhs=wt[:, kt, 0:ncols], start=(kt == 0), stop=(kt == KT - 1))
                return ins
            S.op("pe", fn, reads=self.wbuf[slot] + [self.rb[ti // 2]], writes=[pb])
            evac(ti, pa, pb)

    def rhs_blk(self, kt, b):
        return self.rhs3[:, kt, b * 512:(b + 1) * 512]

    def rhs_blk_bufs(self, b):
        return [self.rb[2 * b], self.rb[2 * b + 1]]

    def rstd_from_ss(self, es_tiles, ss_ap, ss_buf, rs, rsb, n):
        nc, S = self.nc, self.S
        S.op("dve", lambda: nc.vector.tensor_scalar(out=rs, in0=ss_ap, scalar1=1.0 / D, scalar2=EPS,
                                                    op0=ALU.mult, op1=ALU.add), [ss_buf], [rsb])
        S.op("act", lambda: nc.scalar.activation(out=rs, in_=rs, func=AF.Sqrt), [rsb], [rsb])
        S.op("dve", lambda: nc.vector.reciprocal(out=rs, in_=rs), [rsb], [rsb])

    def norm_stage(self, st, gcol, pref=None):
        nc, S, BN = self.nc, self.S, self.BN
        if pref is not None:
            self.prefetch_w(*pref)
        with ExitStack() as es:
            xin = [self.sb(es, f"n_xin{i}", [128, 16, BN], F32) for i in range(2)]
            sqb = [self.sb(es, f"n_sqb{i}", [128, 16, BN], BF16) for i in range(2)]
            rs = [self.sb(es, f"n_rs{i}", [128, BN], F32) for i in range(2)]
            xb = [Buf("xin"), Buf("xin")]
            qb = [Buf("sq"), Buf("sq")]
            rsb = [Buf("rs"), Buf("rs")]
            for b in range(self.TS // BN):
                s = b % 2
                c0 = st * self.TS + b * BN
                S.dma("sp", xin[s][:], self.HT[:, :, c0:c0 + BN].rearrange("k p t -> p k t"), [], [xb[s]])
                S.op("act", lambda: nc.scalar.activation(out=sqb[s][:], in_=xin[s][:], func=AF.Square), [xb[s]], [qb[s]])
                pa, pb = self.lbank[b % 4]

                def fn():
                    ins = None
                    for kt in range(16):
                        ins = nc.tensor.matmul(pa[:, 0:BN], lhsT=self.ones_bf[:], rhs=sqb[s][:, kt, :],
                                               start=(kt == 0), stop=(kt == 15))
                    return ins
                S.op("pe", fn, [qb[s]], [pb])
                self.rstd_from_ss(None, pa[:, 0:BN], pb, rs[s][:], rsb[s], BN)
                for kt in range(16):
                    S.op("dve", lambda: nc.vector.scalar_tensor_tensor(
                        out=self.rhs3[:, kt, b * BN:(b + 1) * BN], in0=xin[s][:, kt, :],
                        scalar=self.gains[:, gcol + kt:gcol + kt + 1], in1=rs[s][:],
                        op0=ALU.mult, op1=ALU.mult), [xb[s], rsb[s]], [self.rb[b]])
            S.barrier()

    def finalize_stage(self, st, parts, gcol):
        nc, S, BN = self.nc, self.S, 128
        with ExitStack() as es:
            ys = [[self.sb(es, f"f_y{p}_{i}", [128, 16, BN], F32) for i in range(2)] for p in range(len(parts))]
            hx = [self.sb(es, f"f_hx{i}", [128, 16, BN], F32) for i in range(2)]
            sqb = [self.sb(es, f"f_sqb{i}", [128, 16, BN], BF16) for i in range(2)]
            rs = [self.sb(es, f"f_rs{i}", [128, BN], F32) for i in range(2)]
            yb = [[Buf("y") for i in range(2)] for p in parts]
            hb = [Buf("hx"), Buf("hx")]
            qb = [Buf("sq"), Buf("sq")]
            rsb = [Buf("rs"), Buf("rs")]
            def loads(b):
                s = b % 2
                c0 = st * self.TS + b * BN
                for p, Y in enumerate(parts):
                    S.dma("sp", ys[p][s][:], Y[:, :, b * BN:(b + 1) * BN].rearrange("k p t -> p k t"),
                          [self.ybuf[p]], [yb[p][s]])
                S.dma("sp", hx[s][:], self.HT[:, :, c0:c0 + BN].rearrange("k p t -> p k t"), [self.htbuf], [hb[s]])
            nblk = self.TS // BN
            loads(0)
            for b in range(nblk):
                s = b % 2
                c0 = st * self.TS + b * BN
                if b + 1 < nblk:
                    loads(b + 1)
                y = ys[0][s]
                for p in range(1, len(parts)):
                    if p == 1:
                        S.op("dve", lambda: nc.vector.tensor_tensor(out=y[:], in0=y[:], in1=ys[p][s][:], op=ALU.add),
                             [yb[0][s], yb[p][s]], [yb[0][s]])
                    else:
                        S.op("pool", lambda: nc.gpsimd.tensor_tensor(out=y[:], in0=y[:], in1=ys[p][s][:], op=ALU.add),
                             [yb[0][s], yb[p][s]], [yb[0][s]])
                S.op("act", lambda: nc.scalar.activation(out=sqb[s][:], in_=y[:], func=AF.Square), [yb[0][s]], [qb[s]])
                pa, pb = self.lbank[b % 4]

                def fn():
                    ins = None
                    for kt in range(16):
                        ins = nc.tensor.matmul(pa[:, 0:BN], lhsT=self.ones_bf[:], rhs=sqb[s][:, kt, :],
                                               start=(kt == 0), stop=(kt == 15))
                    return ins
                S.op("pe", fn, [qb[s]], [pb])
                self.rstd_from_ss(None, pa[:, 0:BN], pb, rs[s][:], rsb[s], BN)
                for kt in range(16):
                    S.op("dve", lambda: nc.vector.scalar_tensor_tensor(
                        out=y[:, kt, :], in0=y[:, kt, :], scalar=self.gains[:, gcol + kt:gcol + kt + 1],
                        in1=rs[s][:], op0=ALU.mult, op1=ALU.mult), [yb[0][s], rsb[s]], [yb[0][s]])
                S.op("pool", lambda: nc.gpsimd.tensor_tensor(out=hx[s][:], in0=hx[s][:], in1=y[:], op=ALU.add),
                     [yb[0][s], hb[s]], [hb[s]])
                S.dma("sp", self.HT[:, :, c0:c0 + BN].rearrange("k p t -> p k t"), hx[s][:], [hb[s]], [self.htbuf])
            S.barrier()

    def proj_out_stage(self, W, Ktiles, src, gt_mode=False):
        nc, S = self.nc, self.S
        splits = [16] * (Ktiles // 16) + ([Ktiles % 16] if Ktiles % 16 else [])
        nparts = len(splits)
        gcols = 512
        with ExitStack() as es:
            ysb = [self.sb(es, f"o_y{i}", [128, 512], F32) for i in range(2)]
            yb = [Buf("ysb"), Buf("ysb")]
            k = [0]
            for p in range(nparts):
                KT = splits[p]
                k0 = 16 * p
                for kt in range(KT):
                    S.dma("sp", self.rhs3[:, kt, :], src[k0 + kt, :, :], [self.srcbuf], self.rb)
                Y = self.Y[p]

                def evac(gi, mi, b, pa, pb):
                    s = k[0] % 2
                    k[0] += 1
                    self.evac_copy(ysb[s][:], pa[:, 0:512], [pb], [yb[s]])
                    dt = gi * (gcols // 128) + mi
                    S.dma("sp", Y[dt, :, b * 512:(b + 1) * 512], ysb[s][:], [yb[s]], [self.ybuf[p]])
                groups = [[(c, gcols)] for c in range(0, D, gcols)]
                self.lin_feat(W, k0 * 128, KT, groups, self.rhs_blk, self.rhs_blk_bufs, evac)
            S.barrier()
        return [self.Y[p] for p in range(nparts)]

    def ffn_up_stage(self, st, l):
        nc, S, TS = self.nc, self.S, self.TS
        W = self.w_up[l]
        with ExitStack() as es:
            UU = [[self.sb(es, f"u_{s}_{m}", [128, TS + 2], F32) for m in range(2)] for s in range(2)]
            ub = [[Buf("uu") for m in range(2)] for s in range(2)]
            cc = [[self.sb(es, f"c_{s}_{m}", [128, TS], F32) for m in range(2)] for s in range(2)]
            cb = [[Buf("cc") for m in range(2)] for s in range(2)]
            gt = [self.sb(es, f"gt_{s}", [128, TS], BF16) for s in range(2)]
            gb = [Buf("gt"), Buf("gt")]
            cwb = self.cw
            banks = list(self.lbank) + [(self.psl(es, "pf0", F32, 512), Buf("pf0")), (self.psl(es, "pf1", F32, 512), Buf("pf1"))]
            state = {"slot": 0, "wt": None, "bk": 0}

            def mm_tile(j):
                g2, jj = j // 2, j % 2
                s = j % 2
                if jj == 0:
                    state["slot"], _ = self.get_w(W, 0, 16, [(g2 * 256, 256), (DFF + g2 * 256, 256)])
                slot = state["slot"]
                wt = self.wslots[slot]
                for m in range(2):
                    S.op("act", lambda: nc.scalar.activation(out=UU[s][m][:, 0:2], in_=self.halo[:, m, j, :], func=AF.Copy),
                         [self.halob], [ub[s][m]])
                for m in range(2):
                    mi = m * 2 + jj
                    for b in range(self.NB):
                        pa, pb = banks[state["bk"] % 6]
                        state["bk"] += 1

                        def fn():
                            ins = None
                            for kt in range(16):
                                ins = nc.tensor.matmul(pa[:, 0:512], lhsT=wt[:, kt, mi * 128:(mi + 1) * 128],
                                                       rhs=self.rhs_blk(kt, b), start=(kt == 0), stop=(kt == 15))
                            return ins
                        S.op("pe", fn, self.wbuf[slot] + self.rhs_blk_bufs(b), [pb])
                        self.evac_copy(UU[s][m][:, 2 + b * 512:2 + (b + 1) * 512], pa[:, 0:512], [pb], [ub[s][m]])

            def conv_tile(j):
                s = j % 2
                for m in range(2):
                    col = m * NFT + j
                    u = UU[s][m]
                    c = cc[s][m]
                    w0 = cwb[:, (l * 3 + 0) * 88 + col:(l * 3 + 0) * 88 + col + 1]
                    w1 = cwb[:, (l * 3 + 1) * 88 + col:(l * 3 + 1) * 88 + col + 1]
                    w2 = cwb[:, (l * 3 + 2) * 88 + col:(l * 3 + 2) * 88 + col + 1]
                    bia = self.cbias[:, l * 88 + col:l * 88 + col + 1]
                    S.op("act", lambda: nc.scalar.activation(out=c[:], in_=u[:, 2:TS + 2], func=AF.Identity,
                                                             bias=bia, scale=w2), [ub[s][m]], [cb[s][m]])
                    S.op("dve", lambda: nc.vector.scalar_tensor_tensor(out=c[:], in0=u[:, 1:TS + 1], scalar=w1, in1=c[:],
                                                                       op0=ALU.mult, op1=ALU.add), [ub[s][m], cb[s][m]], [cb[s][m]])
                    S.op("dve", lambda: nc.vector.scalar_tensor_tensor(out=c[:], in0=u[:, 0:TS], scalar=w0, in1=c[:],
                                                                       op0=ALU.mult, op1=ALU.add), [ub[s][m], cb[s][m]], [cb[s][m]])
                    S.op("act", lambda: nc.scalar.activation(out=self.halo[:, m, j, :], in_=u[:, TS:TS + 2], func=AF.Copy),
                         [ub[s][m]], [self.halob])
                S.op("act", lambda: nc.scalar.activation(out=cc[s][0][:], in_=cc[s][0][:], func=AF.Silu),
                     [cb[s][0]], [cb[s][0]])
                S.op("dve", lambda: nc.vector.tensor_tensor(out=gt[s][:], in0=cc[s][0][:], in1=cc[s][1][:], op=ALU.mult),
                     [cb[s][0], cb[s][1]], [gb[s]])
                S.dma("sp", self.GT[j, :, :], gt[s][:], [gb[s]], [self.srcbuf])

            for j in range(NFT + 1):
                if j < NFT:
                    mm_tile(j)
                if j >= 1:
                    conv_tile(j - 1)
            S.barrier()

    def out_transpose_store(self, ho, hob, c0tile, tok0, hts, htb):
        nc, S = self.nc, self.S
        pa, pb = self.pT[1]

        def fn():
            ins = None
            for c in range(4):
                ins = nc.tensor.transpose(pa[:, c * 128:(c + 1) * 128], ho[:, c * 128:(c + 1) * 128], self.ident[:])
            return ins
        S.op("pe", fn, [hob], [pb])
        S.op("act", lambda: nc.scalar.activation(out=hts[:], in_=pa[:, 0:512], func=AF.Copy), [pb], [htb])
        S.dma("sp", self.HTmix[c0tile:c0tile + 4, :, tok0:tok0 + 128].rearrange("c p t -> p c t"),
              hts[:].rearrange("p (c t) -> p c t", c=4), [htb], [self.srcbuf])

    def out_transpose_store_multi(self, ho, hobs, c0tile, tok0, hts, htb):
        nc, S = self.nc, self.S
        pa, pb = self.pT[1]

        def fn():
            ins = None
            for c in range(4):
                ins = nc.tensor.transpose(pa[:, c * 128:(c + 1) * 128], ho[:, c * 128:(c + 1) * 128], self.ident[:])
            return ins
        S.op("pe", fn, list(hobs), [pb])
        S.op("act", lambda: nc.scalar.activation(out=hts[:], in_=pa[:, 0:512], func=AF.Copy), [pb], [htb])
        S.dma("sp", self.HTmix[c0tile:c0tile + 4, :, tok0:tok0 + 128].rearrange("c p t -> p c t"),
              hts[:].rearrange("p (c t) -> p c t", c=4), [htb], [self.srcbuf])

    def linattn_stage(self, st, kind, j):
        nc, S, TS, NT = self.nc, self.S, self.TS, self.NT
        ml = (kind == 0)
        H = 4 if ml else 8
        W = self.m_w_in[j] if ml else self.r_w_in[j]
        qc, kc, vc, oc = (0, 1024, 2048, 4096) if ml else (0, 2048, 4096, 8192)
        hn = self.m_hn[j] if ml else self.r_hn[j]
        with ExitStack() as es:
            pU_loc = ([self.psl(es, "pu0", F32, 512), self.psl(es, "pu1", F32, 512)], Buf("pU"))
            QT = self.sb(es, "QT", [128, 2, TS], BF16)
            KT_ = self.sb(es, "KT", [128, 2, TS], BF16)
            V = self.sb(es, "V", [128, NT, 512], BF16)
            OG = self.sb(es, "OG", [128, NT, 512], BF16)
            qkb = [Buf("qk") for _ in range(self.NB)]
            vb = [Buf("v") for _ in range(NT)]
            ob = [Buf("og") for _ in range(NT)]
            Cbf = self.sb(es, "Cbf", [128, 2, 512], BF16)
            cbfb = Buf("cbf")
            hng = self.sb(es, "hng", [128, 512], F32)
            hngb = Buf("hng")
            Sw = self.sb(es, "Sw", [128, 128], BF16); swb = Buf("sw")
            kw = self.sb(es, "kw", [128, 256], BF16); kwb = Buf("kw")
            numA = [self.sb(es, f"numA{i}", [128, 512], F32) for i in range(3)]; nab = [Buf("numA") for _ in range(3)]
            numB = [self.sb(es, f"numB{i}", [128, 512], F32) for i in range(3)]; nbb = [Buf("numB") for _ in range(3)]
            Ucp = [self.sb(es, f"Ucp{i}", [128, 2, 512], F32) for i in range(2)]; ucb = [Buf("ucp"), Buf("ucp")]
            num = self.sb(es, "num", [128, 512], F32); numb = Buf("num")
            junk = self.sb(es, "junk", [128, 512], BF16); junkb = Buf("junk")
            hnt = self.sb(es, "hnt", [128, 512], F32); hntb = Buf("hnt")
            ho = self.sb(es, "ho", [128, 512], BF16); hob = Buf("ho")
            hts = self.sb(es, "hts", [128, 512], BF16); htb = Buf("hts")
            sm = self.sb(es, "sm", [128, 16], F32); smb = Buf("sm")
            Cf = self.Cst[:].rearrange("p (a b) -> p a b", a=2)
            if ml:
                G1 = self.sb(es, "G1", [128, NT, 8], F32); g1b = Buf("g1")
                GE = self.sb(es, "GE", [128, NT, 4], F32); geb = Buf("ge")
                LOGF = self.sb(es, "LOGF", [128, NT, 4], F32); lfb = Buf("lf")
                BG = self.sb(es, "BG", [128, NT, 8], F32); bgb = Buf("bg")
                EB = self.sb(es, "EB", [128, NT, 4], F32)
                EG = self.sb(es, "EG", [128, NT, 4], F32)
                WK = self.sb(es, "WK", [128, NT, 4], F32)
                TMP = self.sb(es, "TMPg", [128, NT, 4], F32); tmpb = Buf("tmp")
                gtb = Buf("gates")
                Lm = self.sb(es, "Lm", [128, 128], F32); lmb = Buf("lm")
                Wt = self.sb(es, "Wt", [128, 128], F32); wtb = Buf("wt")
                nbf = self.sb(es, "nbf", [128, 2], BF16); nbfb = Buf("nbf")
                dsb = [self.sb(es, f"dsb{i}", [128, 8], F32) for i in range(3)]; dsbb = [Buf("dsb") for _ in range(3)]
                gbias = self.m_gb[:, j * 8:(j + 1) * 8]

                def gev(ti, pa, pb):
                    S.op("dve", lambda: nc.vector.tensor_tensor(out=G1[:, ti, :], in0=pa[:, 0:8], in1=gbias, op=ALU.add),
                         [pb], [g1b])
                self.lin_tok(W, 0, 16, [(6144, 8)], gev)
                S.op("act", lambda: nc.scalar.activation(out=G1[:], in_=G1[:], func=AF.Tanh, scale=1.0 / 15.0), [g1b], [g1b])
                S.op("dve", lambda: nc.vector.tensor_scalar(out=G1[:], in0=G1[:], scalar1=15.0, scalar2=None, op0=ALU.mult),
                     [g1b], [g1b])
                S.op("act", lambda: nc.scalar.activation(out=GE[:], in_=G1[:, :, 4:8], func=AF.Exp, scale=-1.0), [g1b], [geb])
                S.op("act", lambda: nc.scalar.activation(out=GE[:], in_=GE[:], func=AF.Ln, bias=1.0, scale=1.0), [geb], [geb])
                S.op("dve", lambda: nc.vector.tensor_scalar(out=LOGF[:], in0=GE[:], scalar1=-1.0, scalar2=None, op0=ALU.mult),
                     [geb], [lfb])
                pa, pb = self.pmisc

                def fn():
                    ins = None
                    for ti in range(NT):
                        nc.tensor.matmul(pa[:, ti * 8:ti * 8 + 4], lhsT=self.tri_f[:], rhs=LOGF[:, ti, :], start=True, stop=True)
                        ins = nc.tensor.matmul(pa[:, ti * 8 + 4:ti * 8 + 8], lhsT=self.ones_f[:], rhs=LOGF[:, ti, :],
                                               start=True, stop=True)
                    return ins
                S.op("pe", fn, [lfb], [pb])
                S.op("act", lambda: nc.scalar.activation(out=BG[:].rearrange("p a b -> p (a b)"), in_=pa[:, 0:NT * 8], func=AF.Copy),
                     [pb], [bgb])
                S.op("act", lambda: nc.scalar.activation(out=EB[:], in_=BG[:, :, 0:4], func=AF.Exp, bias=float(math.log(1.0 / 16.0)), scale=1.0), [bgb], [gtb])
                S.op("act", lambda: nc.scalar.activation(out=EG[:], in_=BG[:, :, 4:8], func=AF.Exp), [bgb], [gtb])
                S.op("dve", lambda: nc.vector.tensor_tensor(out=TMP[:], in0=BG[:, :, 4:8], in1=BG[:, :, 0:4], op=ALU.subtract),
                     [bgb], [tmpb])
                S.op("dve", lambda: nc.vector.tensor_tensor(out=TMP[:], in0=TMP[:], in1=G1[:, :, 0:4], op=ALU.add),
                     [tmpb, g1b], [tmpb])
                S.op("act", lambda: nc.scalar.activation(out=WK[:], in_=TMP[:], func=AF.Exp), [tmpb], [gtb])
            else:
                cosT = self.sb(es, "cosT", [128, 512], F32)
                sinT = self.sb(es, "sinT", [128, 512], F32)
                tabb = Buf("tab")
                t1 = self.sb(es, "rt1", [128, 512], F32); t1b = Buf("t1")
                t2 = self.sb(es, "rt2", [128, 512], F32); t2b = Buf("t2")

            for h in range(H):
                if ml:
                    def qk_ev(gi, mi, b, pa, pb):
                        dst = QT if mi < 2 else KT_
                        self.evac_copy(dst[:, mi % 2, b * 512:(b + 1) * 512], pa[:, 0:512], [pb], [qkb[b]])
                    self.lin_feat(W, 0, 16, [[(qc + h * 256, 256), (kc + h * 256, 256)]], self.rhs_blk, self.rhs_blk_bufs, qk_ev)
                else:
                    def qk_evb(gi, b, outs):
                        S.dma("sp", cosT[:], self.cosT_d[:, st * TS + b * 512:st * TS + (b + 1) * 512], [], [tabb])
                        S.dma("sp", sinT[:], self.sinT_d[:, st * TS + b * 512:st * TS + (b + 1) * 512], [], [tabb])
                        cs = cosT[:]
                        sn = sinT[:]
                        for qi, dst in ((0, QT), (1, KT_)):
                            (p0, b0), (p1, b1) = outs[2 * qi], outs[2 * qi + 1]
                            S.op("dve", lambda: nc.vector.tensor_tensor(out=t1[:], in0=p0[:, 0:512], in1=cs, op=ALU.mult), [b0, tabb], [t1b])
                            S.op("dve", lambda: nc.vector.tensor_tensor(out=t2[:], in0=p1[:, 0:512], in1=sn, op=ALU.mult), [b1, tabb], [t2b])
                            S.op("pool", lambda: nc.gpsimd.tensor_tensor(out=dst[:, 0, b * 512:(b + 1) * 512], in0=t1[:], in1=t2[:], op=ALU.subtract),
                                 [t1b, t2b], [qkb[b]])
                            S.op("dve", lambda: nc.vector.tensor_tensor(out=t1[:], in0=p0[:, 0:512], in1=sn, op=ALU.mult), [b0, tabb], [t1b])
                            S.op("dve", lambda: nc.vector.tensor_tensor(out=t2[:], in0=p1[:, 0:512], in1=cs, op=ALU.mult), [b1, tabb], [t2b])
                            S.op("pool", lambda: nc.gpsimd.tensor_tensor(out=dst[:, 1, b * 512:(b + 1) * 512], in0=t1[:], in1=t2[:], op=ALU.add),
                                 [t1b, t2b], [qkb[b]])
                    self.lin_feat(W, 0, 16, [[(qc + h * 256, 256), (kc + h * 256, 256)]], self.rhs_blk, self.rhs_blk_bufs,
                                  qk_evb, block_outer=True)

                def v_ev(ti, pa, pb):
                    self.evac_copy(V[:, ti, :], pa[:, 0:512], [pb], [vb[ti]])
                self.lin_tok(W, 0, 16, [(vc + h * 512, 512)], v_ev)
                gfun = AF.Sigmoid if ml else AF.Silu

                def o_ev(ti, pa, pb):
                    S.op("act", lambda: nc.scalar.activation(out=OG[:, ti, :], in_=pa[:, 0:512], func=gfun), [pb], [ob[ti]])
                self.lin_tok(W, 0, 16, [(oc + h * 512, 512)], o_ev)
                if h + 1 < H:
                    self.prefetch_w(W, 0, 16, [(qc + (h + 1) * 256, 256), (kc + (h + 1) * 256, 256)])
                Cf = self.Cst[:].rearrange("p (a b) -> p a b", a=2)
                S.dma("sp", self.Cst[:], self.CstD[h, :, :], [self.cstdb], [self.cstb])
                S.dma("sp", hng[:], hn[:, h * 512:(h + 1) * 512], [], [hngb])
                S.op("act", lambda: nc.scalar.activation(out=Cbf[:], in_=Cf, func=AF.Copy), [self.cstb], [cbfb])
                if ml:
                    S.op("act", lambda: nc.scalar.activation(out=nbf[:], in_=self.nst[:, h * 2:h * 2 + 2], func=AF.Copy),
                         [self.nstb], [nbfb])
                pS, pSb = self.pS
                pm, pmb = self.pmisc
                pA, pAb = self.pA
                pB, pBb = self.pB
                pK, pKb = self.pT[0]
                pU, pUb = pU_loc
                gsl = hng[:]

                def part_F(ti):
                    p = ti % 2
                    p3 = ti % 3
                    tok = slice(ti * 128, (ti + 1) * 128)
                    qb_ = qkb[ti // 4]

                    def fn():
                        nc.tensor.matmul(pS[:, 0:128], lhsT=KT_[:, 0, tok], rhs=QT[:, 0, tok], start=True, stop=False)
                        return nc.tensor.matmul(pS[:, 0:128], lhsT=KT_[:, 1, tok], rhs=QT[:, 1, tok], start=False, stop=True)
                    S.op("pe", fn, [qb_], [pSb])
                    if ml:
                        S.op("pool", lambda: nc.gpsimd.tensor_scalar(out=Lm[:], in0=self.tri_f[:], scalar1=LOGF[:, ti, h:h + 1],
                                                                     scalar2=1.0, op0=ALU.mult, op1=ALU.mult), [lfb], [lmb])
                        S.op("pe", lambda: nc.tensor.matmul(pm[:, 0:128], lhsT=self.ustr_f[:], rhs=Lm[:], start=True, stop=True),
                             [lmb], [pmb])
                        S.op("act", lambda: nc.scalar.activation(out=Wt[:], in_=pm[:, 0:128], func=AF.Exp,
                                                                 bias=G1[:, ti, h:h + 1], scale=1.0), [pmb, g1b], [wtb])
                        S.op("pool", lambda: nc.gpsimd.tensor_tensor(out=Wt[:], in0=Wt[:], in1=self.mscale[:], op=ALU.mult), [wtb], [wtb])
                        S.op("dve", lambda: nc.vector.tensor_tensor(out=Sw[:], in0=pS[:, 0:128], in1=Wt[:], op=ALU.mult),
                             [pSb, wtb], [swb])
                    else:
                        S.op("dve", lambda: nc.vector.tensor_tensor(out=Sw[:], in0=pS[:, 0:128], in1=self.dmaskT[:, h, :], op=ALU.mult),
                             [pSb], [swb])
                    S.op("pe", lambda: nc.tensor.matmul(pA[:, 0:512], lhsT=Sw[:], rhs=V[:, ti, :], start=True, stop=True),
                         [swb, vb[ti]], [pAb])
                    S.op("act", lambda: nc.scalar.activation(out=numA[p3][:], in_=pA[:, 0:512], func=AF.Copy), [pAb], [nab[p3]])

                    def fn():
                        nc.tensor.transpose(pK[:, 0:128], KT_[:, 0, tok], self.ident[:])
                        return nc.tensor.transpose(pK[:, 128:256], KT_[:, 1, tok], self.ident[:])
                    S.op("pe", fn, [qb_], [pKb])
                    wcol = WK[:, ti, h:h + 1] if ml else self.statew[:, h:h + 1]
                    S.op("dve", lambda: nc.vector.tensor_scalar(out=kw[:], in0=pK[:, 0:256], scalar1=wcol, scalar2=None, op0=ALU.mult),
                         [pKb] + ([gtb] if ml else []), [kwb])

                    def fn():
                        nc.tensor.matmul(pU[0][:, 0:512], lhsT=kw[:, 0:128], rhs=V[:, ti, :], start=True, stop=True)
                        return nc.tensor.matmul(pU[1][:, 0:512], lhsT=kw[:, 128:256], rhs=V[:, ti, :], start=True, stop=True)
                    S.op("pe", fn, [kwb, vb[ti]], [pUb])
                    S.op("act", lambda: nc.scalar.activation(out=Ucp[p][:, 0, :], in_=pU[0][:, 0:512], func=AF.Copy), [pUb], [ucb[p]])
                    S.op("dve", lambda: nc.vector.tensor_copy(out=Ucp[p][:, 1, :], in_=pU[1][:, 0:512]), [pUb], [ucb[p]])
                    if ml:
                        def fn():
                            nc.tensor.matmul(pm[:, 128:129], lhsT=Sw[:], rhs=self.ones_bf[:, 0:1], start=True, stop=True)
                            nc.tensor.matmul(pm[:, 132:133], lhsT=kw[:, 0:128], rhs=self.ones_bf[:, 0:1], start=True, stop=True)
                            return nc.tensor.matmul(pm[:, 133:134], lhsT=kw[:, 128:256], rhs=self.ones_bf[:, 0:1], start=True, stop=True)
                        S.op("pe", fn, [swb, kwb], [pmb])
                        S.op("act", lambda: nc.scalar.activation(out=dsb[p3][:, 0:6], in_=pm[:, 128:134], func=AF.Copy), [pmb], [dsbb[p3]])

                def part_M(ti):
                    p = ti % 2
                    p3 = ti % 3
                    tok = slice(ti * 128, (ti + 1) * 128)
                    qb_ = qkb[ti // 4]

                    def fn():
                        nc.tensor.matmul(pB[:, 0:512], lhsT=QT[:, 0, tok], rhs=Cbf[:, 0, :], start=True, stop=False)
                        return nc.tensor.matmul(pB[:, 0:512], lhsT=QT[:, 1, tok], rhs=Cbf[:, 1, :], start=False, stop=True)
                    S.op("pe", fn, [qb_, cbfb], [pBb])
                    S.op("act", lambda: nc.scalar.activation(out=numB[p3][:], in_=pB[:, 0:512], func=AF.Copy), [pBb], [nbb[p3]])
                    if ml:
                        def fn():
                            nc.tensor.matmul(pm[:, 136:137], lhsT=QT[:, 0, tok], rhs=nbf[:, 0:1], start=True, stop=False)
                            return nc.tensor.matmul(pm[:, 136:137], lhsT=QT[:, 1, tok], rhs=nbf[:, 1:2], start=False, stop=True)
                        S.op("pe", fn, [qb_, nbfb], [pmb])
                        S.op("act", lambda: nc.scalar.activation(out=dsb[p3][:, 6:7], in_=pm[:, 136:137], func=AF.Copy), [pmb], [dsbb[p3]])
                    for jj in range(2):
                        dec = EG[:, ti, h:h + 1] if ml else float(self.gamma_L[h])
                        S.op("dve", lambda: nc.vector.scalar_tensor_tensor(out=Cf[:, jj, :], in0=Cf[:, jj, :], scalar=dec,
                                                                           in1=Ucp[p][:, jj, :], op0=ALU.mult, op1=ALU.add),
                             [self.cstb, ucb[p]] + ([gtb] if ml else []), [self.cstb])
                    S.op("act", lambda: nc.scalar.activation(out=Cbf[:], in_=Cf, func=AF.Copy), [self.cstb], [cbfb])
                    if ml:
                        S.op("dve", lambda: nc.vector.scalar_tensor_tensor(out=self.nst[:, h * 2:h * 2 + 2], in0=self.nst[:, h * 2:h * 2 + 2],
                                                                           scalar=EG[:, ti, h:h + 1], in1=dsb[p3][:, 4:6],
                                                                           op0=ALU.mult, op1=ALU.add), [self.nstb, dsbb[p3], gtb], [self.nstb])
                        S.op("act", lambda: nc.scalar.activation(out=nbf[:], in_=self.nst[:, h * 2:h * 2 + 2], func=AF.Copy),
                             [self.nstb], [nbfb])

                def part_O(ti):
                    p = ti % 3
                    dcol = EB[:, ti, h:h + 1] if ml else self.idec[:, h:h + 1]
                    if ml:
                        S.op("dve", lambda: nc.vector.scalar_tensor_tensor(out=num[:], in0=numB[p][:], scalar=dcol, in1=numA[p][:],
                                                                           op0=ALU.mult, op1=ALU.add), [nbb[p], nab[p], gtb], [numb])
                        S.op("dve", lambda: nc.vector.scalar_tensor_tensor(out=sm[:, 0:1], in0=dsb[p][:, 6:7], scalar=dcol, in1=dsb[p][:, 0:1],
                                                                           op0=ALU.mult, op1=ALU.add), [dsbb[p], gtb], [smb])
                        S.op("act", lambda: nc.scalar.activation(out=junk[:], in_=num[:], func=AF.Square, accum_out=sm[:, 3:4]),
                             [numb], [junkb, smb])
                        S.op("dve", lambda: nc.vector.tensor_tensor(out=sm[:, 1:2], in0=sm[:, 0:1], in1=sm[:, 0:1], op=ALU.mult), [smb], [smb])
                        S.op("dve", lambda: nc.vector.tensor_scalar(out=sm[:, 2:3], in0=sm[:, 1:2], scalar1=1.0, scalar2=EPS,
                                                                    op0=ALU.max, op1=ALU.mult), [smb], [smb])
                        S.op("dve", lambda: nc.vector.scalar_tensor_tensor(out=sm[:, 5:6], in0=sm[:, 3:4], scalar=1.0 / 512, in1=sm[:, 2:3],
                                                                           op0=ALU.mult, op1=ALU.add), [smb], [smb])
                        S.op("act", lambda: nc.scalar.activation(out=sm[:, 5:6], in_=sm[:, 5:6], func=AF.Ln), [smb], [smb])
                        S.op("act", lambda: nc.scalar.activation(out=sm[:, 7:8], in_=sm[:, 5:6], func=AF.Exp, scale=-0.5), [smb], [smb])
                        S.op("dve", lambda: nc.vector.scalar_tensor_tensor(out=hnt[:], in0=num[:], scalar=sm[:, 7:8], in1=gsl,
                                                                           op0=ALU.mult, op1=ALU.mult), [numb, smb, hngb], [hntb])
                    else:
                        S.op("dve", lambda: nc.vector.scalar_tensor_tensor(out=num[:], in0=numB[p][:], scalar=dcol, in1=numA[p][:],
                                                                           op0=ALU.mult, op1=ALU.add, accum_out=sm[:, 0:1]),
                             [nbb[p], nab[p]], [numb, smb])
                        S.op("act", lambda: nc.scalar.activation(out=junk[:], in_=num[:], func=AF.Square, accum_out=sm[:, 1:2]),
                             [numb], [junkb, smb])
                        S.op("dve", lambda: nc.vector.tensor_scalar(out=sm[:, 2:3], in0=sm[:, 0:1], scalar1=1.0 / 512, scalar2=None, op0=ALU.mult), [smb], [smb])
                        S.op("dve", lambda: nc.vector.scalar_tensor_tensor(out=sm[:, 3:4], in0=sm[:, 2:3], scalar=-1.0, in1=sm[:, 2:3],
                                                                           op0=ALU.mult, op1=ALU.mult), [smb], [smb])
                        S.op("dve", lambda: nc.vector.scalar_tensor_tensor(out=sm[:, 4:5], in0=sm[:, 1:2], scalar=1.0 / 512, in1=sm[:, 3:4],
                                                                           op0=ALU.mult, op1=ALU.add), [smb], [smb])
                        S.op("dve", lambda: nc.vector.tensor_scalar(out=sm[:, 4:5], in0=sm[:, 4:5], scalar1=EPS, scalar2=None, op0=ALU.add), [smb], [smb])
                        S.op("act", lambda: nc.scalar.activation(out=sm[:, 4:5], in_=sm[:, 4:5], func=AF.Ln), [smb], [smb])
                        S.op("act", lambda: nc.scalar.activation(out=sm[:, 5:6], in_=sm[:, 4:5], func=AF.Exp, scale=-0.5), [smb], [smb])
                        S.op("dve", lambda: nc.vector.scalar_tensor_tensor(out=sm[:, 6:7], in0=sm[:, 2:3], scalar=-1.0, in1=sm[:, 5:6],
                                                                           op0=ALU.mult, op1=ALU.mult), [smb], [smb])
                        S.op("act", lambda: nc.scalar.activation(out=num[:], in_=num[:], func=AF.Identity, bias=sm[:, 6:7], scale=sm[:, 5:6]),
                             [numb, smb], [numb])
                        S.op("dve", lambda: nc.vector.tensor_tensor(out=hnt[:], in0=num[:], in1=gsl, op=ALU.mult), [numb, hngb], [hntb])
                    S.op("pool", lambda: nc.gpsimd.tensor_tensor(out=ho[:], in0=hnt[:], in1=OG[:, ti, :], op=ALU.mult), [hntb, ob[ti]], [hob])
                    self.out_transpose_store(ho, hob, 4 * h, ti * 128, hts, htb)

                part_F(0)
                if NT > 1:
                    part_F(1)
                part_M(0)
                for ti in range(NT):
                    if ti + 2 < NT:
                        part_F(ti + 2)
                    if ti + 1 < NT:
                        part_M(ti + 1)
                    part_O(ti)
                S.dma("sp", self.CstD[h, :, :], self.Cst[:], [self.cstb], [self.cstdb])
            S.barrier()

    def swa_stage(self, st, j):
        nc, S, TS, NT = self.nc, self.S, self.TS, self.NT
        W = self.s_w_qkv[j]
        with ExitStack() as es:
            QT = self.sb(es, "sQT", [128, 4, TS], BF16)
            KDa = self.sb(es, "sKDa", [128, 128 + TS], BF16)
            KDb = self.sb(es, "sKDb", [128, 128 + TS], BF16)
            VV = self.sb(es, "sVV", [128, NT + 1, 256], BF16)
            qb = [Buf("q") for _ in range(self.NB)]
            kdb = Buf("kd")
            vvb = [Buf("vv") for _ in range(NT + 1)]
            smx = self.sb(es, "ssm", [128, 256], F32); smxb = Buf("smx")
            pr = self.sb(es, "spr", [128, 256], BF16); prb = Buf("pr")
            pT = self.sb(es, "spT", [128, 2, 128], BF16); pTb = Buf("pT")
            sm = self.sb(es, "ssmall", [128, 8], F32); smb = Buf("sm")
            Ot = self.sb(es, "sOt", [128, 512], BF16); otb = Buf("ot")
            hts = self.sb(es, "shts", [128, 512], BF16); htb = Buf("hts")
            otbs = [Buf("ot0"), Buf("ot1")]
            lanes = []
            for ln in range(2):
                lanes.append({
                    "pS": self.lbank[0] if ln == 0 else self.lbank[3],
                    "pA": self.lbank[1] if ln == 0 else self.lbank[2],
                    "pK": self.pT[0] if ln == 0 else (self.psl(es, "ptl1", BF16, 1024), Buf("pK1")),
                    "ko": 0,
                    "smx": (smx, smxb) if ln == 0 else (self.sb(es, "ssm1", [128, 256], F32), Buf("smx1")),
                    "pr": (pr, prb) if ln == 0 else (self.sb(es, "spr1", [128, 256], BF16), Buf("pr1")),
                    "pT": (pT, pTb) if ln == 0 else (self.sb(es, "spT1", [128, 2, 128], BF16), Buf("pT1")),
                    "sm": (sm, smb) if ln == 0 else (self.sb(es, "ssmall1", [128, 8], F32), Buf("sm1")),
                })
            S.op("act", lambda: nc.scalar.activation(out=VV[:, 0, :], in_=self.Vprev[:], func=AF.Copy), [self.vprevb], [vvb[0]])

            def v_ev(ti, pa, pb):
                self.evac_copy(VV[:, ti + 1, :], pa[:, 0:256], [pb], [vvb[ti + 1]])
            self.lin_tok(W, 0, 16, [(2304, 256)], v_ev)
            S.op("act", lambda: nc.scalar.activation(out=self.Vprev[:], in_=VV[:, NT, :], func=AF.Copy), [vvb[NT]], [self.vprevb])
            for g in range(4):
                def q_ev(gi, mi, b, pa, pb):
                    self.evac_copy(QT[:, mi, b * 512:(b + 1) * 512], pa[:, 0:512], [pb], [qb[b]])
                self.lin_feat(W, 0, 16, [[(g * 512, 512)]], self.rhs_blk, self.rhs_blk_bufs, q_ev)
                if g == 0:
                    S.op("pool", lambda: nc.gpsimd.memset(KDa[64:128, :], 0.0), [], [kdb])
                    S.op("pool", lambda: nc.gpsimd.memset(KDb[0:64, :], 0.0), [], [kdb])
                S.op("act", lambda: nc.scalar.activation(out=KDa[0:64, 0:128], in_=self.KTprev[0:64, g, :], func=AF.Copy), [self.kprevb], [kdb])
                S.op("act", lambda: nc.scalar.activation(out=KDb[64:128, 0:128], in_=self.KTprev[64:128, g, :], func=AF.Copy), [self.kprevb], [kdb])

                def k_ev(gi, mi, b, pa, pb):
                    S.op("act", lambda: nc.scalar.activation(out=KDa[0:64, 128 + b * 512:128 + (b + 1) * 512], in_=pa[0:64, 0:512], func=AF.Copy), [pb], [kdb])
                    S.op("dve", lambda: nc.vector.tensor_copy(out=KDb[64:128, 128 + b * 512:128 + (b + 1) * 512], in_=pa[64:128, 0:512]), [pb], [kdb])
                self.lin_feat(W, 0, 16, [[(2048 + g * 64, 64), (2048 + g * 64, 64)]], self.rhs_blk, self.rhs_blk_bufs, k_ev)
                S.op("act", lambda: nc.scalar.activation(out=self.KTprev[0:64, g, :], in_=KDa[0:64, TS:TS + 128], func=AF.Copy), [kdb], [self.kprevb])
                S.op("act", lambda: nc.scalar.activation(out=self.KTprev[64:128, g, :], in_=KDb[64:128, TS:TS + 128], func=AF.Copy), [kdb], [self.kprevb])
                for n in range(NT):
                    first = (st == 0 and n == 0)
                    mask = self.swa_mask[:, 0, :] if first else self.swa_mask[:, 1, :]

                    def chain(hh, lane):
                        head = 8 * g + hh
                        mi, half = hh // 2, hh % 2
                        KDh = KDa if half == 0 else KDb
                        pS, pSb = lanes[lane]["pS"]
                        pK, pKb = lanes[lane]["pK"]
                        ko = lanes[lane]["ko"]
                        pA, pAb = lanes[lane]["pA"]
                        smx_, smxb_ = lanes[lane]["smx"]
                        pr_, prb_ = lanes[lane]["pr"]
                        pT_, pTb_ = lanes[lane]["pT"]
                        sm_, smb_ = lanes[lane]["sm"]
                        S.op("pe", lambda: nc.tensor.matmul(pS[:, 0:256], lhsT=QT[:, mi, n * 128:(n + 1) * 128],
                                                            rhs=KDh[:, n * 128:n * 128 + 256], start=True, stop=True),
                             [qb[n // 4], kdb], [pSb])
                        yield
                        S.op("dve", lambda: nc.vector.scalar_tensor_tensor(out=smx_[:], in0=pS[:, 0:256], scalar=0.125, in1=mask,
                                                                           op0=ALU.mult, op1=ALU.add), [pSb], [smxb_])
                        yield
                        S.op("dve", lambda: nc.vector.reduce_max(out=sm_[:, 0:1], in_=smx_[:], axis=AX.X), [smxb_], [smb_])
                        yield
                        S.op("dve", lambda: nc.vector.tensor_scalar(out=sm_[:, 1:2], in0=sm_[:, 0:1], scalar1=self.sinks[:, head:head + 1],
                                                                    scalar2=-1.0, op0=ALU.max, op1=ALU.mult), [smb_], [smb_])
                        yield
                        S.op("act", lambda: nc.scalar.activation(out=pr_[:], in_=smx_[:], func=AF.Exp, bias=sm_[:, 1:2], scale=1.0,
                                                                 accum_out=sm_[:, 2:3]), [smxb_, smb_], [prb_, smb_])
                        yield
                        S.op("act", lambda: nc.scalar.activation(out=sm_[:, 3:4], in_=self.sinks[:, head:head + 1], func=AF.Exp,
                                                                 bias=sm_[:, 1:2], scale=1.0), [smb_], [smb_])

                        def fn():
                            nc.tensor.transpose(pK[:, ko:ko + 128], pr_[:, 0:128], self.ident[:])
                            return nc.tensor.transpose(pK[:, ko + 128:ko + 256], pr_[:, 128:256], self.ident[:])
                        S.op("pe", fn, [prb_], [pKb])
                        yield
                        S.op("dve", lambda: nc.vector.tensor_tensor(out=sm_[:, 4:5], in0=sm_[:, 2:3], in1=sm_[:, 3:4], op=ALU.add), [smb_], [smb_])
                        self.evac_copy(pT_[:].rearrange("p a b -> p (a b)"), pK[:, ko:ko + 256], [pKb], [pTb_])
                        yield
                        S.op("dve", lambda: nc.vector.reciprocal(out=sm_[:, 5:6], in_=sm_[:, 4:5]), [smb_], [smb_])

                        def fn():
                            nc.tensor.matmul(pA[:, 0:64], lhsT=pT_[:, 0, :], rhs=VV[:, n, g * 64:(g + 1) * 64], start=True, stop=False)
                            return nc.tensor.matmul(pA[:, 0:64], lhsT=pT_[:, 1, :], rhs=VV[:, n + 1, g * 64:(g + 1) * 64], start=False, stop=True)
                        S.op("pe", fn, [pTb_, vvb[n], vvb[n + 1]], [pAb])
                        yield
                        S.op("dve", lambda: nc.vector.tensor_scalar(out=Ot[:, hh * 64:(hh + 1) * 64], in0=pA[:, 0:64], scalar1=sm_[:, 5:6],
                                                                    scalar2=None, op0=ALU.mult), [pAb, smb_], [otbs[hh % 2]])
                        yield

                    for hp in range(4):
                        g0, g1 = chain(2 * hp, 0), chain(2 * hp + 1, 1)
                        alive = [g0, g1]
                        while alive:
                            for gen in list(alive):
                                try:
                                    next(gen)
                                except StopIteration:
                                    alive.remove(gen)
                    self.out_transpose_store_multi(Ot, otbs, 4 * g, n * 128, hts, htb)
            S.barrier()

    def build(self):
        nc, T, TS = self.nc, self.T, self.TS
        es = self.es
        S = self.S = Sched(nc, es)
        self.xT = self.dram("xT", [16, 128, T])
        self.HT = self.dram("outT", [16, 128, T], kind="ExternalOutput")
        self.gains_d = self.dram("gains", [128, 256])
        self.w_up = self.dram("w_up", [4, D, 2 * DFF])
        self.cw_d = self.dram("conv_w", [128, 4 * 3 * 88])
        self.cb_d = self.dram("conv_b", [128, 4 * 88])
        self.w_down = self.dram("w_down", [4, DFF, D])
        self.m_w_in = self.dram("m_w_in", [2, D, 6152])
        self.m_gb_d = self.dram("m_gb", [128, 16])
        self.m_hn = self.dram("m_hn", [2, 128, 2048])
        self.m_w_out = self.dram("m_w_out", [2, D, D])
        self.s_w_qkv = self.dram("s_w_qkv", [1, D, 2560])
        self.sinks_d = self.dram("s_sinks", [128, 32])
        self.s_w_out = self.dram("s_w_out", [1, D, D])
        self.r_w_in = self.dram("r_w_in", [1, D, 12288])
        self.r_hn = self.dram("r_hn", [1, 128, 4096])
        self.r_w_out = self.dram("r_w_out", [1, 4096, D])
        cst = {n: self.dram(n, shp) for n, shp in [("c_ident", [128, 128]), ("c_ones", [128, 128]), ("c_tri", [128, 128]),
                                                    ("c_ustr", [128, 128]), ("c_mscale", [128, 128]), ("c_swamask", [128, 512]),
                                                    ("c_dmaskT", [128, 8 * 128]), ("c_idec", [128, 8]), ("c_statew", [128, 8])]}
        self.cosT_d = self.dram("c_cosT", [128, T])
        self.sinT_d = self.dram("c_sinT", [128, T])
        self.Y = [self.dram(f"Y{p}", [16, 128, TS], kind="Internal") for p in range(3)]
        self.CstD = self.dram("CstD", [8, 128, 1024], kind="Internal")
        self.HTmix = self.dram("HTmix", [32, 128, TS], BF16, kind="Internal")
        self.GT = self.dram("GT", [NFT, 128, TS], BF16, kind="Internal")
        self.ybuf = [Buf("Y0"), Buf("Y1"), Buf("Y2")]
        self.cstdb = Buf("CstD")
        self.htbuf = Buf("HT")
        self.srcbuf = Buf("src")
        lg = np.log(1.0 - np.power(2.0, -5.0 - np.arange(8, dtype=np.float64)))
        self.gamma_L = np.exp(lg * 128.0)
        sb = lambda n, s, d: self.sb(es, n, s, d)
        self.ident = sb("ident", [128, 128], BF16)
        self.ones_bf = sb("ones_bf", [128, 128], BF16)
        self.ones_f = sb("ones_f", [128, 128], F32)
        self.tri_f = sb("tri_f", [128, 128], F32)
        self.ustr_f = sb("ustr_f", [128, 128], F32)
        self.mscale = sb("mscale", [128, 128], F32)
        self.swa_mask = sb("swa_mask", [128, 2, 256], F32)
        self.dmaskT = sb("dmaskT", [128, 8, 128], F32)
        self.idec = sb("idec", [128, 8], F32)
        self.statew = sb("statew", [128, 8], F32)
        self.gains = sb("gains_sb", [128, 256], F32)
        self.cw = sb("cw_sb", [128, 4 * 3 * 88], F32)
        self.cbias = sb("cb_sb", [128, 4 * 88], F32)
        self.m_gb = sb("m_gb_sb", [128, 16], F32)
        self.sinks = sb("sinks_sb", [128, 32], F32)
        self.rhs3 = sb("rhsbuf", [128, 16, TS], BF16)
        self.rb = [Buf(f"rhs{b}") for b in range(TS // 256)]
        self.wslots = [sb(f"wslot{i}", [128, 16, 512], BF16) for i in range(2)]
        self.wbuf = [[Buf("w0a"), Buf("w0b")], [Buf("w1a"), Buf("w1b")]]
        self.pref = None
        self.wnext = 0
        self.lnext = 0
        self.Cst = sb("Cst", [128, 1024], F32)
        self.cstb = Buf("Cst")
        self.nst = sb("nst", [128, 8], F32)
        self.nstb = Buf("nst")
        self.KTprev = sb("KTprev", [128, 4, 128], BF16)
        self.kprevb = Buf("kprev")
        self.Vprev = sb("Vprev", [128, 256], BF16)
        self.vprevb = Buf("vprev")
        self.halo = sb("halo", [128, 2, NFT, 2], F32)
        self.halob = Buf("halo")
        ps = lambda n, dt, w: es.enter_context(nc.psum_tensor(n, [128, w], dt))
        self.lbank = [(ps(f"pl{i}", F32, 512), Buf(f"pl{i}")) for i in range(4)]
        self.pS = self.lbank[0]
        self.pA = self.lbank[1]
        self.pB = self.lbank[2]
        self.pmisc = self.lbank[3]
        self.pT = [(ps("pt0", BF16, 1024), Buf("pt0")), (ps("pt1", BF16, 1024), Buf("pt1"))]
        cb = Buf("consts")
        for dst, src in [(self.ident, "c_ident"), (self.ones_bf, "c_ones")]:
            S.dma("pool", dst[:], cst[src][:, :], [], [cb])
        for dst, src in [(self.ones_f, "c_ones"), (self.tri_f, "c_tri"), (self.ustr_f, "c_ustr"), (self.mscale, "c_mscale"),
                         (self.idec, "c_idec"), (self.statew, "c_statew")]:
            S.dma("sp", dst[:], cst[src][:, :], [], [cb])
        S.dma("sp", self.swa_mask[:].rearrange("p a b -> p (a b)"), cst["c_swamask"][:, :], [], [cb])
        S.dma("sp", self.dmaskT[:].rearrange("p a b -> p (a b)"), cst["c_dmaskT"][:, :], [], [cb])
        S.dma("sp", self.gains[:], self.gains_d[:, :], [], [cb])
        S.dma("sp", self.cw[:], self.cw_d[:, :], [], [cb])
        S.dma("sp", self.cbias[:], self.cb_d[:, :], [], [cb])
        S.dma("sp", self.m_gb[:], self.m_gb_d[:, :], [], [cb])
        S.dma("sp", self.sinks[:], self.sinks_d[:, :], [], [cb])
        for k in range(16):
            S.dma("sp", self.HT[k, :, :], self.xT[k, :, :], [], [self.htbuf])
        S.barrier()
        for l in self.layers:
            kind, j = l % 3, l // 3
            S.op("pool", lambda: nc.gpsimd.memset(self.Cst[:], 0.0), [], [self.cstb])
            for hh in range(8):
                S.dma("sp", self.CstD[hh, :, :], self.Cst[:], [self.cstb], [self.cstdb])
            S.op("pool", lambda: nc.gpsimd.memset(self.nst[:], 0.0), [], [self.nstb])
            S.op("pool", lambda: nc.gpsimd.memset(self.KTprev[:], 0.0), [], [self.kprevb])
            S.op("pool", lambda: nc.gpsimd.memset(self.Vprev[:], 0.0), [], [self.vprevb])
            S.op("pool", lambda: nc.gpsimd.memset(self.halo[:], 0.0), [], [self.halob])
            import os as _os
            mx = int(_os.environ.get("MAXSTAGE", "1000"))
            for st in range(self.NST):
                pf = None
                if kind == 1:
                    pf = (self.s_w_qkv[j], 0, 16, [(2304, 256)])
                elif kind == 2:
                    pf = (self.r_w_in[j], 0, 16, [(0, 256), (2048, 256)])
                if self.stg < mx: self.norm_stage(st, (l * 4 + 0) * 16, pf)
                self.stg += 1
                if kind == 1:
                    if self.stg < mx: self.swa_stage(st, j)
                    Wo, Kt = self.s_w_out[j], 16
                else:
                    if self.stg < mx: self.linattn_stage(st, kind, j)
                    Wo, Kt = (self.m_w_out[j], 16) if kind == 0 else (self.r_w_out[j], 32)
                self.stg += 1
                if self.stg < mx: parts = self.proj_out_stage(Wo, Kt, self.HTmix)
                self.stg += 1
                if self.stg < mx: self.finalize_stage(st, parts, (l * 4 + 1) * 16)
                self.stg += 1
                if self.stg < mx: self.norm_stage(st, (l * 4 + 2) * 16, (self.w_up[l], 0, 16, [(0, 256), (DFF, 256)]))
                self.stg += 1
                if self.stg < mx: self.ffn_up_stage(st, l)
                self.stg += 1
                if self.stg < mx: parts = self.proj_out_stage(self.w_down[l], NFT, self.GT)
                self.stg += 1
                if self.stg < mx: self.finalize_stage(st, parts, (l * 4 + 3) * 16)
                self.stg += 1
        S.barrier()
        self.es.close()
        return nc


def host_consts(T):
    c = {}
    i = np.arange(128)
    c["c_ident"] = np.eye(128, dtype=np.float32)
    c["c_ones"] = np.ones((128, 128), np.float32)
    c["c_tri"] = (i[:, None] <= i[None, :]).astype(np.float32)
    c["c_ustr"] = (i[:, None] > i[None, :]).astype(np.float32)
    c["c_mscale"] = (i[:, None] <= i[None, :]).astype(np.float32) * np.float32(256 ** -0.5)
    q = i[:, None]
    jj = np.arange(256)[None, :]
    valid = (jj > q) & (jj <= q + 128)
    m1 = np.where(valid, 0.0, -30000.0).astype(np.float32)
    m0 = np.where(valid & (jj >= 128), 0.0, -30000.0).astype(np.float32)
    c["c_swamask"] = np.concatenate([m0, m1], axis=1)
    lg = np.log(1.0 - np.power(2.0, -5.0 - np.arange(8, dtype=np.float64)))
    rel = (i[None, :] - i[:, None]).astype(np.float64)
    dm = np.where(rel[None] >= 0, np.exp(lg[:, None, None] * np.maximum(rel[None], 0.0)), 0.0) * (256 ** -0.5)
    c["c_dmaskT"] = np.ascontiguousarray(dm.transpose(1, 0, 2).reshape(128, 8 * 128)).astype(np.float32)
    c["c_idec"] = np.exp(lg[None, :] * (i[:, None] + 1.0)).astype(np.float32)
    c["c_statew"] = (np.exp(lg[None, :] * (127.0 - i[:, None])) * (256 ** -0.5)).astype(np.float32)
    half = 128
    inv = np.power(np.float32(10000.0), -np.arange(half, dtype=np.float32) / np.float32(half)).astype(np.float32)
    pos = np.arange(T, dtype=np.float32)
    ang = (pos[None, :] * inv[:, None]).astype(np.float32)
    c["c_cosT"] = np.cos(ang).astype(np.float32)
    c["c_sinT"] = np.sin(ang).astype(np.float32)
    return c


def host_layout(inp):
    o = {}
    g = inp["norm_gains"].reshape(4, 4, 16, 128)
    o["gains"] = np.ascontiguousarray(g.transpose(3, 0, 1, 2).reshape(128, 256))
    cw = inp["ffn_conv_w"].reshape(4, 3, 88, 128)
    o["conv_w"] = np.ascontiguousarray(cw.transpose(3, 0, 1, 2).reshape(128, 4 * 3 * 88))
    cbv = inp["ffn_conv_b"].reshape(4, 88, 128)
    o["conv_b"] = np.ascontiguousarray(cbv.transpose(2, 0, 1).reshape(128, 4 * 88))
    o["w_up"] = inp["ffn_w_up"]
    o["w_down"] = inp["ffn_w_down"]
    o["m_w_in"] = inp["mlstm_w_in"]
    gb = inp["mlstm_gate_b"].reshape(2, 8)
    o["m_gb"] = np.ascontiguousarray(np.broadcast_to(gb.reshape(1, 16), (128, 16)))
    o["m_hn"] = np.ascontiguousarray(np.broadcast_to(inp["mlstm_head_norm"][:, None, :], (2, 128, 2048)))
    o["m_w_out"] = inp["mlstm_w_out"]
    o["s_w_qkv"] = inp["swa_w_qkv"]
    o["s_sinks"] = np.ascontiguousarray(np.broadcast_to(inp["swa_sinks"].reshape(1, 32), (128, 32)))
    o["s_w_out"] = inp["swa_w_out"]
    o["r_w_in"] = inp["ret_w_in"]
    o["r_hn"] = np.ascontiguousarray(np.broadcast_to(inp["ret_head_norm"][:, None, :], (1, 128, 4096)))
    o["r_w_out"] = inp["ret_w_out"]
    return {k: np.ascontiguousarray(v, dtype=np.float32) for k, v in o.items()}


def run(inp, T, TS, layers, ncores):
    x = inp["x"]
    b = Builder(T, TS, layers)
    nc = b.build()
    shared = host_layout(inp)
    shared.update(host_consts(T))
    in_maps = []
    for c in range(ncores):
        m = dict(shared)
        xc = x[c]
        m["xT"] = np.ascontiguousarray(xc.T.reshape(16, 128, T))
        in_maps.append(m)
    res = run_bass_kernel_spmd(nc, in_maps, core_ids=list(range(ncores)))
    outs = []
    for c in range(ncores):
        oT = res.results[c]["outT"]
        outs.append(np.ascontiguousarray(oT.reshape(2048, T).T))
    return np.stack(outs, axis=0).astype(np.float32)


def kernel(**inputs):
    inp = {k: np.asarray(v) for k, v in inputs.items()}
    return run(inp, 4096, 2048, [0, 1, 2, 3], 4)
```

```python
import math
from contextlib import ExitStack
import numpy as np
import concourse.bass as bass
import concourse.mybir as mybir
from concourse.bass_utils import run_bass_kernel_spmd

F32 = mybir.dt.float32
BF16 = mybir.dt.bfloat16
AF = mybir.ActivationFunctionType
ALU = mybir.AluOpType
AX = mybir.AxisListType

D = 2048
DFF = 5632
NFT = DFF // 128
EPS = 1e-6
NSD = 12


class Buf:
    __slots__ = ("name", "w", "r")

    def __init__(self, name):
        self.name = name
        self.w = None
        self.r = []


class Sched:
    ENG = ["pe", "act", "dve", "pool", "sp"]

    def __init__(self, nc, es):
        self.nc = nc
        self.h = {"pe": nc.tensor, "act": nc.scalar, "dve": nc.vector, "pool": nc.gpsimd, "sp": nc.sync}
        self.sem = {e: es.enter_context(nc.semaphore("c_" + e)) for e in self.ENG}
        self.cnt = {e: 0 for e in self.ENG}
        self.dsem = {e: [es.enter_context(nc.semaphore(f"d_{e}{i}")) for i in range(NSD)] for e in ("sp", "pool")}
        self.ndma = {"sp": 0, "pool": 0}
        self.wc = {e: {} for e in self.ENG}
        self.wd = {e: set() for e in self.ENG}

    def _wait(self, eng, tok):
        kind, e2, v = tok
        E = self.h[eng]
        if kind == "c":
            if e2 == eng and eng == "pe":
                return
            if self.wc[eng].get(e2, 0) >= v:
                return
            self.wc[eng][e2] = v
            E.wait_ge(self.sem[e2], v)
        else:
            if tok in self.wd[eng]:
                return
            self.wd[eng].add(tok)
            E.wait_ge(self.dsem[e2][v % NSD], 16 * (v // NSD + 1))

    def _deps(self, eng, reads, writes):
        deps = []
        for b in reads:
            if b.w is not None:
                deps.append(b.w)
        for b in writes:
            if b.w is not None:
                deps.append(b.w)
            deps.extend(b.r)
        best = {}
        for d in deps:
            if d[0] == "c":
                if best.get(d[1], 0) < d[2]:
                    best[d[1]] = d[2]
            else:
                self._wait(eng, d)
        for e2, v in best.items():
            self._wait(eng, ("c", e2, v))

    def _mark(self, tok, reads, writes):
        for b in reads:
            b.r.append(tok)
        for b in writes:
            b.w = tok
            b.r = []

    def op(self, eng, fn, reads=(), writes=()):
        self._deps(eng, reads, writes)
        ins = fn()
        self.cnt[eng] += 1
        ins.then_inc(self.sem[eng], 1)
        tok = ("c", eng, self.cnt[eng])
        self._mark(tok, reads, writes)
        return tok

    def dma(self, eng, out, in_, reads=(), writes=()):
        k = self.ndma[eng]
        self.ndma[eng] += 1
        if k >= NSD:
            self._wait(eng, ("d", eng, k - NSD))
        self._deps(eng, reads, writes)
        ins = self.h[eng].dma_start(out=out, in_=in_)
        ins.then_inc(self.dsem[eng][k % NSD], 16)
        tok = ("d", eng, k)
        self._mark(tok, reads, writes)
        return tok

    def barrier(self):
        toks = []
        for e in self.ENG:
            if self.cnt[e] > 0:
                toks.append(("c", e, self.cnt[e]))
        for e in ("sp", "pool"):
            for k in range(max(0, self.ndma[e] - NSD), self.ndma[e]):
                toks.append(("d", e, k))
        for e in self.ENG:
            for t in toks:
                if t[0] == "c" and t[1] == e:
                    if self.wc[e].get(e, 0) < t[2]:
                        self.wc[e][e] = t[2]
                        self.h[e].wait_ge(self.sem[e], t[2])
                    continue
                self._wait(e, t)


class Builder:
    def __init__(self, T, TS, layers):
        self.T, self.TS, self.layers = T, TS, layers
        self.NT = TS // 128
        self.NB = TS // 512
        self.NST = T // TS
        self.BN = 256
        self.nc = bass.Bass("TRN2", target_bir_lowering=False)
        self.es = ExitStack()
        self.evk = 0
        self.stg = 0

    def dram(self, name, shape, dt=F32, kind="ExternalInput"):
        return self.nc.dram_tensor(name, list(shape), dt, kind=kind).ap()

    def psl(self, es, name, dt, w):
        self.uid = getattr(self, "uid", 0) + 1
        return es.enter_context(self.nc.psum_tensor(f"{name}_{self.uid}", [128, w], dt))

    def sb(self, es, name, shape, dt):
        self.uid = getattr(self, "uid", 0) + 1
        return es.enter_context(self.nc.sbuf_tensor(f"{name}_{self.uid}", list(shape), dt))

    def evac_copy(self, out, in_, reads, writes):
        self.evk += 1
        if self.evk % 2 == 0:
            self.S.op("act", lambda: self.nc.scalar.activation(out=out, in_=in_, func=AF.Copy), reads, writes)
        else:
            self.S.op("dve", lambda: self.nc.vector.tensor_copy(out=out, in_=in_), reads, writes)

    def load_w(self, slot, W, row0, KT, segs):
        wt = self.wslots[slot]
        off = 0
        for i, (c0, n) in enumerate(segs):
            src = W[row0:row0 + KT * 128, c0:c0 + n].rearrange("(kt p) c -> p kt c", p=128)
            self.S.dma("pool", wt[:, 0:KT, off:off + n], src, reads=(), writes=(self.wbuf[slot][i],))
            off += n
        return off

    def get_w(self, W, row0, KT, segs):
        key = (id(W.tensor) if hasattr(W, "tensor") else 0, str(W), row0, KT, tuple(segs))
        if self.pref is not None and self.pref[0] == key:
            slot, ncols = self.pref[1], self.pref[2]
            self.pref = None
            return slot, ncols
        assert self.pref is None, "unused prefetch"
        slot = self.wnext % 2
        self.wnext += 1
        return slot, self.load_w(slot, W, row0, KT, segs)

    def prefetch_w(self, W, row0, KT, segs):
        key = (id(W.tensor) if hasattr(W, "tensor") else 0, str(W), row0, KT, tuple(segs))
        slot = self.wnext % 2
        self.wnext += 1
        self.pref = (key, slot, self.load_w(slot, W, row0, KT, segs))

    def lin_feat(self, W, row0, KT, groups, rhs_ap, rhs_bufs, evac, block_outer=False, ncolmax=512, banks=None):
        nc, S = self.nc, self.S
        for gi, segs in enumerate(groups):
            slot, ncols = self.get_w(W, row0, KT, segs)
            wt = self.wslots[slot]
            nm = ncols // 128

            bankl = banks if banks is not None else self.lbank

            def group(mi, b, bank):
                pa, pb = bankl[bank]

                def fn():
                    ins = None
                    for kt in range(KT):
                        ins = nc.tensor.matmul(pa[:, 0:512], lhsT=wt[:, kt, mi * 128:(mi + 1) * 128],
                                               rhs=rhs_ap(kt, b), start=(kt == 0), stop=(kt == KT - 1))
                    return ins
                S.op("pe", fn, reads=self.wbuf[slot] + rhs_bufs(b), writes=[pb])
                return pa, pb

            if block_outer:
                for b in range(self.NB):
                    outs = []
                    for mi in range(nm):
                        outs.append(group(mi, b, mi % 4))
                    evac(gi, b, outs)
            else:
                for mi in range(nm):
                    for b in range(self.NB):
                        bank = self.lnext % len(bankl)
                        self.lnext += 1
                        pa, pb = group(mi, b, bank)
                        evac(gi, mi, b, pa, pb)

    def lin_tok(self, W, row0, KT, segs, evac):
        nc, S = self.nc, self.S
        slot, ncols = self.get_w(W, row0, KT, segs)
        wt = self.wslots[slot]
        for ti in range(self.NT):
            bank = self.lnext % 4
            self.lnext += 1
            pa, pb = self.lbank[bank]

            def fn():
                ins = None
                for kt in range(KT):
                    ins = nc.tensor.matmul(pa[:, 0:ncols], lhsT=self.rhs3[:, kt, ti * 128:(ti + 1) * 128],
                                           rhs=wt[:, kt, 0:ncols], start=(kt == 0), stop=(kt == KT - 1))
                return ins
            S.op("pe", fn, reads=self.wbuf[slot] + [self.rb[ti // 2]], writes=[pb])
            evac(ti, pa, pb)

    def rhs_blk(self, kt, b):
        return self.rhs3[:, kt, b * 512:(b + 1) * 512]

    def rhs_blk_bufs(self, b):
        return [self.rb[2 * b], self.rb[2 * b + 1]]

    def rstd_from_ss(self, es_tiles, ss_ap, ss_buf, rs, rsb, n):
        nc, S = self.nc, self.S
        S.op("dve", lambda: nc.vector.tensor_scalar(out=rs, in0=ss_ap, scalar1=1.0 / D, scalar2=EPS,
                                                    op0=ALU.mult, op1=ALU.add), [ss_buf], [rsb])
        S.op("act", lambda: nc.scalar.activation(out=rs, in_=rs, func=AF.Sqrt), [rsb], [rsb])
        S.op("dve", lambda: nc.vector.reciprocal(out=rs, in_=rs), [rsb], [rsb])

    def norm_stage(self, st, gcol, pref=None):
        nc, S, BN = self.nc, self.S, self.BN
        if pref is not None:
            self.prefetch_w(*pref)
        with ExitStack() as es:
            xin = [self.sb(es, f"n_xin{i}", [128, 16, BN], F32) for i in range(2)]
            sqb = [self.sb(es, f"n_sqb{i}", [128, 16, BN], BF16) for i in range(2)]
            rs = [self.sb(es, f"n_rs{i}", [128, BN], F32) for i in range(2)]
            xb = [Buf("xin"), Buf("xin")]
            qb = [Buf("sq"), Buf("sq")]
            rsb = [Buf("rs"), Buf("rs")]
            for b in range(self.TS // BN):
                s = b % 2
                c0 = st * self.TS + b * BN
                S.dma("sp", xin[s][:], self.HT[:, :, c0:c0 + BN].rearrange("k p t -> p k t"), [], [xb[s]])
                S.op("act", lambda: nc.scalar.activation(out=sqb[s][:], in_=xin[s][:], func=AF.Square), [xb[s]], [qb[s]])
                pa, pb = self.lbank[b % 4]

                def fn():
                    ins = None
                    for kt in range(16):
                        ins = nc.tensor.matmul(pa[:, 0:BN], lhsT=self.ones_bf[:], rhs=sqb[s][:, kt, :],
                                               start=(kt == 0), stop=(kt == 15))
                    return ins
                S.op("pe", fn, [qb[s]], [pb])
                self.rstd_from_ss(None, pa[:, 0:BN], pb, rs[s][:], rsb[s], BN)
                for kt in range(16):
                    S.op("dve", lambda: nc.vector.scalar_tensor_tensor(
                        out=self.rhs3[:, kt, b * BN:(b + 1) * BN], in0=xin[s][:, kt, :],
                        scalar=self.gains[:, gcol + kt:gcol + kt + 1], in1=rs[s][:],
                        op0=ALU.mult, op1=ALU.mult), [xb[s], rsb[s]], [self.rb[b]])
            S.barrier()

    def finalize_stage(self, st, parts, gcol):
        nc, S, BN = self.nc, self.S, 128
        with ExitStack() as es:
            ys = [[self.sb(es, f"f_y{p}_{i}", [128, 16, BN], F32) for i in range(2)] for p in range(len(parts))]
            hx = [self.sb(es, f"f_hx{i}", [128, 16, BN], F32) for i in range(2)]
            sqb = [self.sb(es, f"f_sqb{i}", [128, 16, BN], BF16) for i in range(2)]
            rs = [self.sb(es, f"f_rs{i}", [128, BN], F32) for i in range(2)]
            yb = [[Buf("y") for i in range(2)] for p in parts]
            hb = [Buf("hx"), Buf("hx")]
            qb = [Buf("sq"), Buf("sq")]
            rsb = [Buf("rs"), Buf("rs")]
            def loads(b):
                s = b % 2
                c0 = st * self.TS + b * BN
                for p, Y in enumerate(parts):
                    S.dma("sp", ys[p][s][:], Y[:, :, b * BN:(b + 1) * BN].rearrange("k p t -> p k t"),
                          [self.ybuf[p]], [yb[p][s]])
                S.dma("sp", hx[s][:], self.HT[:, :, c0:c0 + BN].rearrange("k p t -> p k t"), [self.htbuf], [hb[s]])
            nblk = self.TS // BN
            loads(0)
            for b in range(nblk):
                s = b % 2
                c0 = st * self.TS + b * BN
                if b + 1 < nblk:
                    loads(b + 1)
                y = ys[0][s]
                for p in range(1, len(parts)):
                    if p == 1:
                        S.op("dve", lambda: nc.vector.tensor_tensor(out=y[:], in0=y[:], in1=ys[p][s][:], op=ALU.add),
                             [yb[0][s], yb[p][s]], [yb[0][s]])
                    else:
                        S.op("pool", lambda: nc.gpsimd.tensor_tensor(out=y[:], in0=y[:], in1=ys[p][s][:], op=ALU.add),
                             [yb[0][s], yb[p][s]], [yb[0][s]])
                S.op("act", lambda: nc.scalar.activation(out=sqb[s][:], in_=y[:], func=AF.Square), [yb[0][s]], [qb[s]])
                pa, pb = self.lbank[b % 4]

                def fn():
                    ins = None
                    for kt in range(16):
                        ins = nc.tensor.matmul(pa[:, 0:BN], lhsT=self.ones_bf[:], rhs=sqb[s][:, kt, :],
                                               start=(kt == 0), stop=(kt == 15))
                    return ins
                S.op("pe", fn, [qb[s]], [pb])
                self.rstd_from_ss(None, pa[:, 0:BN], pb, rs[s][:], rsb[s], BN)
                for kt in range(16):
                    S.op("dve", lambda: nc.vector.scalar_tensor_tensor(
                        out=y[:, kt, :], in0=y[:, kt, :], scalar=self.gains[:, gcol + kt:gcol + kt + 1],
                        in1=rs[s][:], op0=ALU.mult, op1=ALU.mult), [yb[0][s], rsb[s]], [yb[0][s]])
                S.op("pool", lambda: nc.gpsimd.tensor_tensor(out=hx[s][:], in0=hx[s][:], in1=y[:], op=ALU.add),
                     [yb[0][s], hb[s]], [hb[s]])
                S.dma("sp", self.HT[:, :, c0:c0 + BN].rearrange("k p t -> p k t"), hx[s][:], [hb[s]], [self.htbuf])
            S.barrier()

    def proj_out_stage(self, W, Ktiles, src, gt_mode=False):
        nc, S = self.nc, self.S
        splits = [16] * (Ktiles // 16) + ([Ktiles % 16] if Ktiles % 16 else [])
        nparts = len(splits)
        gcols = 512
        with ExitStack() as es:
            ysb = [self.sb(es, f"o_y{i}", [128, 512], F32) for i in range(4)]
            yb = [Buf("ysb") for _ in range(4)]
            banks6 = list(self.lbank) + [(self.psl(es, "po0", F32, 512), Buf("po0")), (self.psl(es, "po1", F32, 512), Buf("po1"))]
            k = [0]
            for p in range(nparts):
                KT = splits[p]
                k0 = 16 * p
                for kt in range(KT):
                    S.dma("sp", self.rhs3[:, kt, :], src[k0 + kt, :, :], [self.srcbuf], self.rb)
                Y = self.Y[p]

                def evac(gi, mi, b, pa, pb):
                    s = k[0] % 4
                    k[0] += 1
                    self.evac_copy(ysb[s][:], pa[:, 0:512], [pb], [yb[s]])
                    dt = gi * (gcols // 128) + mi
                    S.dma("sp", Y[dt, :, b * 512:(b + 1) * 512], ysb[s][:], [yb[s]], [self.ybuf[p]])
                groups = [[(c, gcols)] for c in range(0, D, gcols)]
                self.lin_feat(W, k0 * 128, KT, groups, self.rhs_blk, self.rhs_blk_bufs, evac, banks=banks6)
            S.barrier()
        return [self.Y[p] for p in range(nparts)]

    def ffn_up_stage(self, st, l):
        nc, S, TS = self.nc, self.S, self.TS
        W = self.w_up[l]
        with ExitStack() as es:
            UU = [[self.sb(es, f"u_{s}_{m}", [128, TS + 2], F32) for m in range(2)] for s in range(2)]
            ub = [[Buf("uu") for m in range(2)] for s in range(2)]
            cc = [[self.sb(es, f"c_{s}_{m}", [128, TS], F32) for m in range(2)] for s in range(2)]
            cb = [[Buf("cc") for m in range(2)] for s in range(2)]
            gt = [self.sb(es, f"gt_{s}", [128, TS], BF16) for s in range(2)]
            gb = [Buf("gt"), Buf("gt")]
            cwb = self.cw
            banks = list(self.lbank) + [(self.psl(es, "pf0", F32, 512), Buf("pf0")), (self.psl(es, "pf1", F32, 512), Buf("pf1"))]
            state = {"slot": 0, "wt": None, "bk": 0}

            def mm_tile(j):
                g2, jj = j // 2, j % 2
                s = j % 2
                if jj == 0:
                    state["slot"], _ = self.get_w(W, 0, 16, [(g2 * 256, 256), (DFF + g2 * 256, 256)])
                slot = state["slot"]
                wt = self.wslots[slot]
                for m in range(2):
                    S.op("act", lambda: nc.scalar.activation(out=UU[s][m][:, 0:2], in_=self.halo[:, m, j, :], func=AF.Copy),
                         [self.halob], [ub[s][m]])
                for m in range(2):
                    mi = m * 2 + jj
                    for b in range(self.NB):
                        pa, pb = banks[state["bk"] % 6]
                        state["bk"] += 1

                        def fn():
                            ins = None
                            for kt in range(16):
                                ins = nc.tensor.matmul(pa[:, 0:512], lhsT=wt[:, kt, mi * 128:(mi + 1) * 128],
                                                       rhs=self.rhs_blk(kt, b), start=(kt == 0), stop=(kt == 15))
                            return ins
                        S.op("pe", fn, self.wbuf[slot] + self.rhs_blk_bufs(b), [pb])
                        self.evac_copy(UU[s][m][:, 2 + b * 512:2 + (b + 1) * 512], pa[:, 0:512], [pb], [ub[s][m]])

            def conv_tile(j):
                s = j % 2
                for m in range(2):
                    col = m * NFT + j
                    u = UU[s][m]
                    c = cc[s][m]
                    w0 = cwb[:, (l * 3 + 0) * 88 + col:(l * 3 + 0) * 88 + col + 1]
                    w1 = cwb[:, (l * 3 + 1) * 88 + col:(l * 3 + 1) * 88 + col + 1]
                    w2 = cwb[:, (l * 3 + 2) * 88 + col:(l * 3 + 2) * 88 + col + 1]
                    bia = self.cbias[:, l * 88 + col:l * 88 + col + 1]
                    S.op("act", lambda: nc.scalar.activation(out=c[:], in_=u[:, 2:TS + 2], func=AF.Identity,
                                                             bias=bia, scale=w2), [ub[s][m]], [cb[s][m]])
                    S.op("dve", lambda: nc.vector.scalar_tensor_tensor(out=c[:], in0=u[:, 1:TS + 1], scalar=w1, in1=c[:],
                                                                       op0=ALU.mult, op1=ALU.add), [ub[s][m], cb[s][m]], [cb[s][m]])
                    S.op("dve", lambda: nc.vector.scalar_tensor_tensor(out=c[:], in0=u[:, 0:TS], scalar=w0, in1=c[:],
                                                                       op0=ALU.mult, op1=ALU.add), [ub[s][m], cb[s][m]], [cb[s][m]])
                    S.op("act", lambda: nc.scalar.activation(out=self.halo[:, m, j, :], in_=u[:, TS:TS + 2], func=AF.Copy),
                         [ub[s][m]], [self.halob])
                S.op("act", lambda: nc.scalar.activation(out=cc[s][0][:], in_=cc[s][0][:], func=AF.Silu),
                     [cb[s][0]], [cb[s][0]])
                S.op("dve", lambda: nc.vector.tensor_tensor(out=gt[s][:], in0=cc[s][0][:], in1=cc[s][1][:], op=ALU.mult),
                     [cb[s][0], cb[s][1]], [gb[s]])
                S.dma("sp", self.GT[j, :, :], gt[s][:], [gb[s]], [self.srcbuf])

            for j in range(NFT + 1):
                if j < NFT:
                    mm_tile(j)
                if j >= 1:
                    conv_tile(j - 1)
            S.barrier()

    def out_transpose_store(self, ho, hob, c0tile, tok0, hts, htb):
        nc, S = self.nc, self.S
        pa, pb = self.pT[1]

        def fn():
            ins = None
            for c in range(4):
                ins = nc.tensor.transpose(pa[:, c * 128:(c + 1) * 128], ho[:, c * 128:(c + 1) * 128], self.ident[:])
            return ins
        S.op("pe", fn, [hob], [pb])
        S.op("act", lambda: nc.scalar.activation(out=hts[:], in_=pa[:, 0:512], func=AF.Copy), [pb], [htb])
        S.dma("sp", self.HTmix[c0tile:c0tile + 4, :, tok0:tok0 + 128].rearrange("c p t -> p c t"),
              hts[:].rearrange("p (c t) -> p c t", c=4), [htb], [self.srcbuf])

    def out_transpose_store_multi(self, ho, hobs, c0tile, tok0, hts, htb):
        nc, S = self.nc, self.S
        pa, pb = self.pT[1]

        def fn():
            ins = None
            for c in range(4):
                ins = nc.tensor.transpose(pa[:, c * 128:(c + 1) * 128], ho[:, c * 128:(c + 1) * 128], self.ident[:])
            return ins
        S.op("pe", fn, list(hobs), [pb])
        S.op("act", lambda: nc.scalar.activation(out=hts[:], in_=pa[:, 0:512], func=AF.Copy), [pb], [htb])
        S.dma("sp", self.HTmix[c0tile:c0tile + 4, :, tok0:tok0 + 128].rearrange("c p t -> p c t"),
              hts[:].rearrange("p (c t) -> p c t", c=4), [htb], [self.srcbuf])

    def linattn_stage(self, st, kind, j):
        nc, S, TS, NT = self.nc, self.S, self.TS, self.NT
        ml = (kind == 0)
        H = 4 if ml else 8
        W = self.m_w_in[j] if ml else self.r_w_in[j]
        qc, kc, vc, oc = (0, 1024, 2048, 4096) if ml else (0, 2048, 4096, 8192)
        hn = self.m_hn[j] if ml else self.r_hn[j]
        with ExitStack() as es:
            pU_loc = ([self.psl(es, "pu0", F32, 512), self.psl(es, "pu1", F32, 512)], Buf("pU"))
            QT = self.sb(es, "QT", [128, 2, TS], BF16)
            KT_ = self.sb(es, "KT", [128, 2, TS], BF16)
            V = self.sb(es, "V", [128, NT, 512], BF16)
            OG = self.sb(es, "OG", [128, NT, 512], BF16)
            qkb = [Buf("qk") for _ in range(self.NB)]
            vb = [Buf("v") for _ in range(NT)]
            ob = [Buf("og") for _ in range(NT)]
            Cbf = self.sb(es, "Cbf", [128, 2, 512], BF16)
            cbfb = Buf("cbf")
            hng = self.sb(es, "hng", [128, 512], F32)
            hngb = Buf("hng")
            Sw = self.sb(es, "Sw", [128, 128], BF16); swb = Buf("sw")
            kw = self.sb(es, "kw", [128, 256], BF16); kwb = Buf("kw")
            numA = [self.sb(es, f"numA{i}", [128, 512], F32) for i in range(3)]; nab = [Buf("numA") for _ in range(3)]
            numB = [self.sb(es, f"numB{i}", [128, 512], F32) for i in range(3)]; nbb = [Buf("numB") for _ in range(3)]
            Ucp = [self.sb(es, f"Ucp{i}", [128, 2, 512], F32) for i in range(2)]; ucb = [Buf("ucp"), Buf("ucp")]
            num = self.sb(es, "num", [128, 512], F32); numb = Buf("num")
            junk = self.sb(es, "junk", [128, 512], BF16); junkb = Buf("junk")
            hnt = self.sb(es, "hnt", [128, 512], F32); hntb = Buf("hnt")
            ho = self.sb(es, "ho", [128, 512], BF16); hob = Buf("ho")
            hts = self.sb(es, "hts", [128, 512], BF16); htb = Buf("hts")
            sm = self.sb(es, "sm", [128, 16], F32); smb = Buf("sm")
            Cf = self.Cst[:].rearrange("p (a b) -> p a b", a=2)
            if ml:
                G1 = self.sb(es, "G1", [128, NT, 8], F32); g1b = Buf("g1")
                GE = self.sb(es, "GE", [128, NT, 4], F32); geb = Buf("ge")
                LOGF = self.sb(es, "LOGF", [128, NT, 4], F32); lfb = Buf("lf")
                BG = self.sb(es, "BG", [128, NT, 8], F32); bgb = Buf("bg")
                EB = self.sb(es, "EB", [128, NT, 4], F32)
                EG = self.sb(es, "EG", [128, NT, 4], F32)
                WK = self.sb(es, "WK", [128, NT, 4], F32)
                TMP = self.sb(es, "TMPg", [128, NT, 4], F32); tmpb = Buf("tmp")
                gtb = Buf("gates")
                Lm = self.sb(es, "Lm", [128, 128], F32); lmb = Buf("lm")
                Wt = self.sb(es, "Wt", [128, 128], F32); wtb = Buf("wt")
                nbf = self.sb(es, "nbf", [128, 2], BF16); nbfb = Buf("nbf")
                dsb = [self.sb(es, f"dsb{i}", [128, 8], F32) for i in range(3)]; dsbb = [Buf("dsb") for _ in range(3)]
                gbias = self.m_gb[:, j * 8:(j + 1) * 8]

                def gev(ti, pa, pb):
                    S.op("dve", lambda: nc.vector.tensor_tensor(out=G1[:, ti, :], in0=pa[:, 0:8], in1=gbias, op=ALU.add),
                         [pb], [g1b])
                self.lin_tok(W, 0, 16, [(6144, 8)], gev)
                S.op("act", lambda: nc.scalar.activation(out=G1[:], in_=G1[:], func=AF.Tanh, scale=1.0 / 15.0), [g1b], [g1b])
                S.op("dve", lambda: nc.vector.tensor_scalar(out=G1[:], in0=G1[:], scalar1=15.0, scalar2=None, op0=ALU.mult),
                     [g1b], [g1b])
                S.op("act", lambda: nc.scalar.activation(out=GE[:], in_=G1[:, :, 4:8], func=AF.Exp, scale=-1.0), [g1b], [geb])
                S.op("act", lambda: nc.scalar.activation(out=GE[:], in_=GE[:], func=AF.Ln, bias=1.0, scale=1.0), [geb], [geb])
                S.op("dve", lambda: nc.vector.tensor_scalar(out=LOGF[:], in0=GE[:], scalar1=-1.0, scalar2=None, op0=ALU.mult),
                     [geb], [lfb])
                pa, pb = self.pmisc

                def fn():
                    ins = None
                    for ti in range(NT):
                        nc.tensor.matmul(pa[:, ti * 8:ti * 8 + 4], lhsT=self.tri_f[:], rhs=LOGF[:, ti, :], start=True, stop=True)
                        ins = nc.tensor.matmul(pa[:, ti * 8 + 4:ti * 8 + 8], lhsT=self.ones_f[:], rhs=LOGF[:, ti, :],
                                               start=True, stop=True)
                    return ins
                S.op("pe", fn, [lfb], [pb])
                S.op("act", lambda: nc.scalar.activation(out=BG[:].rearrange("p a b -> p (a b)"), in_=pa[:, 0:NT * 8], func=AF.Copy),
                     [pb], [bgb])
                S.op("act", lambda: nc.scalar.activation(out=EB[:], in_=BG[:, :, 0:4], func=AF.Exp, bias=float(math.log(1.0 / 16.0)), scale=1.0), [bgb], [gtb])
                S.op("act", lambda: nc.scalar.activation(out=EG[:], in_=BG[:, :, 4:8], func=AF.Exp), [bgb], [gtb])
                S.op("dve", lambda: nc.vector.tensor_tensor(out=TMP[:], in0=BG[:, :, 4:8], in1=BG[:, :, 0:4], op=ALU.subtract),
                     [bgb], [tmpb])
                S.op("dve", lambda: nc.vector.tensor_tensor(out=TMP[:], in0=TMP[:], in1=G1[:, :, 0:4], op=ALU.add),
                     [tmpb, g1b], [tmpb])
                S.op("act", lambda: nc.scalar.activation(out=WK[:], in_=TMP[:], func=AF.Exp), [tmpb], [gtb])
            else:
                cosT = self.sb(es, "cosT", [128, 512], F32)
                sinT = self.sb(es, "sinT", [128, 512], F32)
                tabb = Buf("tab")
                t1 = self.sb(es, "rt1", [128, 512], F32); t1b = Buf("t1")
                t2 = self.sb(es, "rt2", [128, 512], F32); t2b = Buf("t2")

            for h in range(H):
                if ml:
                    def qk_ev(gi, mi, b, pa, pb):
                        dst = QT if mi < 2 else KT_
                        self.evac_copy(dst[:, mi % 2, b * 512:(b + 1) * 512], pa[:, 0:512], [pb], [qkb[b]])
                    self.lin_feat(W, 0, 16, [[(qc + h * 256, 256), (kc + h * 256, 256)]], self.rhs_blk, self.rhs_blk_bufs, qk_ev)
                else:
                    def qk_evb(gi, b, outs):
                        S.dma("sp", cosT[:], self.cosT_d[:, st * TS + b * 512:st * TS + (b + 1) * 512], [], [tabb])
                        S.dma("sp", sinT[:], self.sinT_d[:, st * TS + b * 512:st * TS + (b + 1) * 512], [], [tabb])
                        cs = cosT[:]
                        sn = sinT[:]
                        for qi, dst in ((0, QT), (1, KT_)):
                            (p0, b0), (p1, b1) = outs[2 * qi], outs[2 * qi + 1]
                            S.op("dve", lambda: nc.vector.tensor_tensor(out=t1[:], in0=p0[:, 0:512], in1=cs, op=ALU.mult), [b0, tabb], [t1b])
                            S.op("dve", lambda: nc.vector.tensor_tensor(out=t2[:], in0=p1[:, 0:512], in1=sn, op=ALU.mult), [b1, tabb], [t2b])
                            S.op("pool", lambda: nc.gpsimd.tensor_tensor(out=dst[:, 0, b * 512:(b + 1) * 512], in0=t1[:], in1=t2[:], op=ALU.subtract),
                                 [t1b, t2b], [qkb[b]])
                            S.op("dve", lambda: nc.vector.tensor_tensor(out=t1[:], in0=p0[:, 0:512], in1=sn, op=ALU.mult), [b0, tabb], [t1b])
                            S.op("dve", lambda: nc.vector.tensor_tensor(out=t2[:], in0=p1[:, 0:512], in1=cs, op=ALU.mult), [b1, tabb], [t2b])
                            S.op("pool", lambda: nc.gpsimd.tensor_tensor(out=dst[:, 1, b * 512:(b + 1) * 512], in0=t1[:], in1=t2[:], op=ALU.add),
                                 [t1b, t2b], [qkb[b]])
                    self.lin_feat(W, 0, 16, [[(qc + h * 256, 256), (kc + h * 256, 256)]], self.rhs_blk, self.rhs_blk_bufs,
                                  qk_evb, block_outer=True)

                def v_ev(ti, pa, pb):
                    self.evac_copy(V[:, ti, :], pa[:, 0:512], [pb], [vb[ti]])
                self.lin_tok(W, 0, 16, [(vc + h * 512, 512)], v_ev)
                gfun = AF.Sigmoid if ml else AF.Silu

                def o_ev(ti, pa, pb):
                    S.op("act", lambda: nc.scalar.activation(out=OG[:, ti, :], in_=pa[:, 0:512], func=gfun), [pb], [ob[ti]])
                self.lin_tok(W, 0, 16, [(oc + h * 512, 512)], o_ev)
                if h + 1 < H:
                    self.prefetch_w(W, 0, 16, [(qc + (h + 1) * 256, 256), (kc + (h + 1) * 256, 256)])
                Cf = self.Cst[:].rearrange("p (a b) -> p a b", a=2)
                S.dma("sp", self.Cst[:], self.CstD[h, :, :], [self.cstdb], [self.cstb])
                S.dma("sp", hng[:], hn[:, h * 512:(h + 1) * 512], [], [hngb])
                S.op("act", lambda: nc.scalar.activation(out=Cbf[:], in_=Cf, func=AF.Copy), [self.cstb], [cbfb])
                if ml:
                    S.op("act", lambda: nc.scalar.activation(out=nbf[:], in_=self.nst[:, h * 2:h * 2 + 2], func=AF.Copy),
                         [self.nstb], [nbfb])
                pS, pSb = self.pS
                pm, pmb = self.pmisc
                pA, pAb = self.pA
                pB, pBb = self.pB
                pK, pKb = self.pT[0]
                pU, pUb = pU_loc
                gsl = hng[:]

                def part_F(ti):
                    p = ti % 2
                    p3 = ti % 3
                    tok = slice(ti * 128, (ti + 1) * 128)
                    qb_ = qkb[ti // 4]

                    def fn():
                        nc.tensor.matmul(pS[:, 0:128], lhsT=KT_[:, 0, tok], rhs=QT[:, 0, tok], start=True, stop=False)
                        return nc.tensor.matmul(pS[:, 0:128], lhsT=KT_[:, 1, tok], rhs=QT[:, 1, tok], start=False, stop=True)
                    S.op("pe", fn, [qb_], [pSb])
                    if ml:
                        S.op("pool", lambda: nc.gpsimd.tensor_scalar(out=Lm[:], in0=self.tri_f[:], scalar1=LOGF[:, ti, h:h + 1],
                                                                     scalar2=1.0, op0=ALU.mult, op1=ALU.mult), [lfb], [lmb])
                        S.op("pe", lambda: nc.tensor.matmul(pm[:, 0:128], lhsT=self.ustr_f[:], rhs=Lm[:], start=True, stop=True),
                             [lmb], [pmb])
                        S.op("act", lambda: nc.scalar.activation(out=Wt[:], in_=pm[:, 0:128], func=AF.Exp,
                                                                 bias=G1[:, ti, h:h + 1], scale=1.0), [pmb, g1b], [wtb])
                        S.op("pool", lambda: nc.gpsimd.tensor_tensor(out=Wt[:], in0=Wt[:], in1=self.mscale[:], op=ALU.mult), [wtb], [wtb])
                        S.op("dve", lambda: nc.vector.tensor_tensor(out=Sw[:], in0=pS[:, 0:128], in1=Wt[:], op=ALU.mult),
                             [pSb, wtb], [swb])
                    else:
                        S.op("dve", lambda: nc.vector.tensor_tensor(out=Sw[:], in0=pS[:, 0:128], in1=self.dmaskT[:, h, :], op=ALU.mult),
                             [pSb], [swb])
                    S.op("pe", lambda: nc.tensor.matmul(pA[:, 0:512], lhsT=Sw[:], rhs=V[:, ti, :], start=True, stop=True),
                         [swb, vb[ti]], [pAb])
                    S.op("act", lambda: nc.scalar.activation(out=numA[p3][:], in_=pA[:, 0:512], func=AF.Copy), [pAb], [nab[p3]])

                    def fn():
                        nc.tensor.transpose(pK[:, 0:128], KT_[:, 0, tok], self.ident[:])
                        return nc.tensor.transpose(pK[:, 128:256], KT_[:, 1, tok], self.ident[:])
                    S.op("pe", fn, [qb_], [pKb])
                    wcol = WK[:, ti, h:h + 1] if ml else self.statew[:, h:h + 1]
                    S.op("dve", lambda: nc.vector.tensor_scalar(out=kw[:], in0=pK[:, 0:256], scalar1=wcol, scalar2=None, op0=ALU.mult),
                         [pKb] + ([gtb] if ml else []), [kwb])

                    def fn():
                        nc.tensor.matmul(pU[0][:, 0:512], lhsT=kw[:, 0:128], rhs=V[:, ti, :], start=True, stop=True)
                        return nc.tensor.matmul(pU[1][:, 0:512], lhsT=kw[:, 128:256], rhs=V[:, ti, :], start=True, stop=True)
                    S.op("pe", fn, [kwb, vb[ti]], [pUb])
                    S.op("act", lambda: nc.scalar.activation(out=Ucp[p][:, 0, :], in_=pU[0][:, 0:512], func=AF.Copy), [pUb], [ucb[p]])
                    S.op("dve", lambda: nc.vector.tensor_copy(out=Ucp[p][:, 1, :], in_=pU[1][:, 0:512]), [pUb], [ucb[p]])
                    if ml:
                        def fn():
                            nc.tensor.matmul(pm[:, 128:129], lhsT=Sw[:], rhs=self.ones_bf[:, 0:1], start=True, stop=True)
                            nc.tensor.matmul(pm[:, 132:133], lhsT=kw[:, 0:128], rhs=self.ones_bf[:, 0:1], start=True, stop=True)
                            return nc.tensor.matmul(pm[:, 133:134], lhsT=kw[:, 128:256], rhs=self.ones_bf[:, 0:1], start=True, stop=True)
                        S.op("pe", fn, [swb, kwb], [pmb])
                        S.op("act", lambda: nc.scalar.activation(out=dsb[p3][:, 0:6], in_=pm[:, 128:134], func=AF.Copy), [pmb], [dsbb[p3]])

                def part_M(ti):
                    p = ti % 2
                    p3 = ti % 3
                    tok = slice(ti * 128, (ti + 1) * 128)
                    qb_ = qkb[ti // 4]

                    def fn():
                        nc.tensor.matmul(pB[:, 0:512], lhsT=QT[:, 0, tok], rhs=Cbf[:, 0, :], start=True, stop=False)
                        return nc.tensor.matmul(pB[:, 0:512], lhsT=QT[:, 1, tok], rhs=Cbf[:, 1, :], start=False, stop=True)
                    S.op("pe", fn, [qb_, cbfb], [pBb])
                    S.op("act", lambda: nc.scalar.activation(out=numB[p3][:], in_=pB[:, 0:512], func=AF.Copy), [pBb], [nbb[p3]])
                    if ml:
                        def fn():
                            nc.tensor.matmul(pm[:, 136:137], lhsT=QT[:, 0, tok], rhs=nbf[:, 0:1], start=True, stop=False)
                            return nc.tensor.matmul(pm[:, 136:137], lhsT=QT[:, 1, tok], rhs=nbf[:, 1:2], start=False, stop=True)
                        S.op("pe", fn, [qb_, nbfb], [pmb])
                        S.op("act", lambda: nc.scalar.activation(out=dsb[p3][:, 6:7], in_=pm[:, 136:137], func=AF.Copy), [pmb], [dsbb[p3]])
                    for jj in range(2):
                        dec = EG[:, ti, h:h + 1] if ml else float(self.gamma_L[h])
                        S.op("dve", lambda: nc.vector.scalar_tensor_tensor(out=Cf[:, jj, :], in0=Cf[:, jj, :], scalar=dec,
                                                                           in1=Ucp[p][:, jj, :], op0=ALU.mult, op1=ALU.add),
                             [self.cstb, ucb[p]] + ([gtb] if ml else []), [self.cstb])
                    S.op("act", lambda: nc.scalar.activation(out=Cbf[:], in_=Cf, func=AF.Copy), [self.cstb], [cbfb])
                    if ml:
                        S.op("dve", lambda: nc.vector.scalar_tensor_tensor(out=self.nst[:, h * 2:h * 2 + 2], in0=self.nst[:, h * 2:h * 2 + 2],
                                                                           scalar=EG[:, ti, h:h + 1], in1=dsb[p3][:, 4:6],
                                                                           op0=ALU.mult, op1=ALU.add), [self.nstb, dsbb[p3], gtb], [self.nstb])
                        S.op("act", lambda: nc.scalar.activation(out=nbf[:], in_=self.nst[:, h * 2:h * 2 + 2], func=AF.Copy),
                             [self.nstb], [nbfb])

                def part_O(ti):
                    p = ti % 3
                    dcol = EB[:, ti, h:h + 1] if ml else self.idec[:, h:h + 1]
                    if ml:
                        S.op("dve", lambda: nc.vector.scalar_tensor_tensor(out=num[:], in0=numB[p][:], scalar=dcol, in1=numA[p][:],
                                                                           op0=ALU.mult, op1=ALU.add), [nbb[p], nab[p], gtb], [numb])
                        S.op("dve", lambda: nc.vector.scalar_tensor_tensor(out=sm[:, 0:1], in0=dsb[p][:, 6:7], scalar=dcol, in1=dsb[p][:, 0:1],
                                                                           op0=ALU.mult, op1=ALU.add), [dsbb[p], gtb], [smb])
                        S.op("act", lambda: nc.scalar.activation(out=junk[:], in_=num[:], func=AF.Square, accum_out=sm[:, 3:4]),
                             [numb], [junkb, smb])
                        S.op("dve", lambda: nc.vector.tensor_tensor(out=sm[:, 1:2], in0=sm[:, 0:1], in1=sm[:, 0:1], op=ALU.mult), [smb], [smb])
                        S.op("dve", lambda: nc.vector.tensor_scalar(out=sm[:, 2:3], in0=sm[:, 1:2], scalar1=1.0, scalar2=EPS,
                                                                    op0=ALU.max, op1=ALU.mult), [smb], [smb])
                        S.op("dve", lambda: nc.vector.scalar_tensor_tensor(out=sm[:, 5:6], in0=sm[:, 3:4], scalar=1.0 / 512, in1=sm[:, 2:3],
                                                                           op0=ALU.mult, op1=ALU.add), [smb], [smb])
                        S.op("act", lambda: nc.scalar.activation(out=sm[:, 5:6], in_=sm[:, 5:6], func=AF.Ln), [smb], [smb])
                        S.op("act", lambda: nc.scalar.activation(out=sm[:, 7:8], in_=sm[:, 5:6], func=AF.Exp, scale=-0.5), [smb], [smb])
                        S.op("dve", lambda: nc.vector.scalar_tensor_tensor(out=hnt[:], in0=num[:], scalar=sm[:, 7:8], in1=gsl,
                                                                           op0=ALU.mult, op1=ALU.mult), [numb, smb, hngb], [hntb])
                    else:
                        S.op("dve", lambda: nc.vector.scalar_tensor_tensor(out=num[:], in0=numB[p][:], scalar=dcol, in1=numA[p][:],
                                                                           op0=ALU.mult, op1=ALU.add, accum_out=sm[:, 0:1]),
                             [nbb[p], nab[p]], [numb, smb])
                        S.op("act", lambda: nc.scalar.activation(out=junk[:], in_=num[:], func=AF.Square, accum_out=sm[:, 1:2]),
                             [numb], [junkb, smb])
                        S.op("dve", lambda: nc.vector.tensor_scalar(out=sm[:, 2:3], in0=sm[:, 0:1], scalar1=1.0 / 512, scalar2=None, op0=ALU.mult), [smb], [smb])
                        S.op("dve", lambda: nc.vector.scalar_tensor_tensor(out=sm[:, 3:4], in0=sm[:, 2:3], scalar=-1.0, in1=sm[:, 2:3],
                                                                           op0=ALU.mult, op1=ALU.mult), [smb], [smb])
                        S.op("dve", lambda: nc.vector.scalar_tensor_tensor(out=sm[:, 4:5], in0=sm[:, 1:2], scalar=1.0 / 512, in1=sm[:, 3:4],
                                                                           op0=ALU.mult, op1=ALU.add), [smb], [smb])
                        S.op("dve", lambda: nc.vector.tensor_scalar(out=sm[:, 4:5], in0=sm[:, 4:5], scalar1=EPS, scalar2=None, op0=ALU.add), [smb], [smb])
                        S.op("act", lambda: nc.scalar.activation(out=sm[:, 4:5], in_=sm[:, 4:5], func=AF.Ln), [smb], [smb])
                        S.op("act", lambda: nc.scalar.activation(out=sm[:, 5:6], in_=sm[:, 4:5], func=AF.Exp, scale=-0.5), [smb], [smb])
                        S.op("dve", lambda: nc.vector.scalar_tensor_tensor(out=sm[:, 6:7], in0=sm[:, 2:3], scalar=-1.0, in1=sm[:, 5:6],
                                                                           op0=ALU.mult, op1=ALU.mult), [smb], [smb])
                        S.op("act", lambda: nc.scalar.activation(out=num[:], in_=num[:], func=AF.Identity, bias=sm[:, 6:7], scale=sm[:, 5:6]),
                             [numb, smb], [numb])
                        S.op("dve", lambda: nc.vector.tensor_tensor(out=hnt[:], in0=num[:], in1=gsl, op=ALU.mult), [numb, hngb], [hntb])
                    S.op("pool", lambda: nc.gpsimd.tensor_tensor(out=ho[:], in0=hnt[:], in1=OG[:, ti, :], op=ALU.mult), [hntb, ob[ti]], [hob])
                    self.out_transpose_store(ho, hob, 4 * h, ti * 128, hts, htb)

                part_F(0)
                if NT > 1:
                    part_F(1)
                part_M(0)
                for ti in range(NT):
                    if ti + 2 < NT:
                        part_F(ti + 2)
                    if ti + 1 < NT:
                        part_M(ti + 1)
                    part_O(ti)
                S.dma("sp", self.CstD[h, :, :], self.Cst[:], [self.cstb], [self.cstdb])
            S.barrier()

    def swa_stage(self, st, j):
        nc, S, TS, NT = self.nc, self.S, self.TS, self.NT
        W = self.s_w_qkv[j]
        with ExitStack() as es:
            QT = self.sb(es, "sQT", [128, 4, TS], BF16)
            KDa = self.sb(es, "sKDa", [128, 128 + TS], BF16)
            KDb = self.sb(es, "sKDb", [128, 128 + TS], BF16)
            VV = self.sb(es, "sVV", [128, NT + 1, 256], BF16)
            qb = [Buf("q") for _ in range(self.NB)]
            kdb = Buf("kd")
            vvb = [Buf("vv") for _ in range(NT + 1)]
            smx = self.sb(es, "ssm", [128, 256], F32); smxb = Buf("smx")
            pr = self.sb(es, "spr", [128, 256], BF16); prb = Buf("pr")
            pT = self.sb(es, "spT", [128, 2, 128], BF16); pTb = Buf("pT")
            sm = self.sb(es, "ssmall", [128, 8], F32); smb = Buf("sm")
            Ot = self.sb(es, "sOt", [128, 512], BF16); otb = Buf("ot")
            hts = self.sb(es, "shts", [128, 512], BF16); htb = Buf("hts")
            otbs = [Buf("ot0"), Buf("ot1"), Buf("ot2"), Buf("ot3")]
            lanes = []
            pkb = [self.pT[0], (self.psl(es, "ptl1", BF16, 1024), Buf("pK1")), (self.psl(es, "ptl2", BF16, 1024), Buf("pK2")), self.pT[1]]
            for ln in range(4):
                lanes.append({
                    "pS": self.lbank[ln],
                    "pA": self.lbank[ln],
                    "pK": pkb[ln],
                    "ko": 0,
                    "smx": (smx, smxb) if ln == 0 else (self.sb(es, "ssm1", [128, 256], F32), Buf("smx1")),
                    "pr": (pr, prb) if ln == 0 else (self.sb(es, "spr1", [128, 256], BF16), Buf("pr1")),
                    "pT": (pT, pTb) if ln == 0 else (self.sb(es, "spT1", [128, 2, 128], BF16), Buf("pT1")),
                    "sm": (sm, smb) if ln == 0 else (self.sb(es, "ssmall1", [128, 8], F32), Buf("sm1")),
                })
            S.op("act", lambda: nc.scalar.activation(out=VV[:, 0, :], in_=self.Vprev[:], func=AF.Copy), [self.vprevb], [vvb[0]])

            def v_ev(ti, pa, pb):
                self.evac_copy(VV[:, ti + 1, :], pa[:, 0:256], [pb], [vvb[ti + 1]])
            self.lin_tok(W, 0, 16, [(2304, 256)], v_ev)
            S.op("act", lambda: nc.scalar.activation(out=self.Vprev[:], in_=VV[:, NT, :], func=AF.Copy), [vvb[NT]], [self.vprevb])
            for g in range(4):
                def q_ev(gi, mi, b, pa, pb):
                    self.evac_copy(QT[:, mi, b * 512:(b + 1) * 512], pa[:, 0:512], [pb], [qb[b]])
                self.lin_feat(W, 0, 16, [[(g * 512, 512)]], self.rhs_blk, self.rhs_blk_bufs, q_ev)
                if g == 0:
                    S.op("pool", lambda: nc.gpsimd.memset(KDa[64:128, :], 0.0), [], [kdb])
                    S.op("pool", lambda: nc.gpsimd.memset(KDb[0:64, :], 0.0), [], [kdb])
                S.op("act", lambda: nc.scalar.activation(out=KDa[0:64, 0:128], in_=self.KTprev[0:64, g, :], func=AF.Copy), [self.kprevb], [kdb])
                S.op("act", lambda: nc.scalar.activation(out=KDb[64:128, 0:128], in_=self.KTprev[64:128, g, :], func=AF.Copy), [self.kprevb], [kdb])

                def k_ev(gi, mi, b, pa, pb):
                    S.op("act", lambda: nc.scalar.activation(out=KDa[0:64, 128 + b * 512:128 + (b + 1) * 512], in_=pa[0:64, 0:512], func=AF.Copy), [pb], [kdb])
                    S.op("dve", lambda: nc.vector.tensor_copy(out=KDb[64:128, 128 + b * 512:128 + (b + 1) * 512], in_=pa[64:128, 0:512]), [pb], [kdb])
                self.lin_feat(W, 0, 16, [[(2048 + g * 64, 64), (2048 + g * 64, 64)]], self.rhs_blk, self.rhs_blk_bufs, k_ev)
                S.op("act", lambda: nc.scalar.activation(out=self.KTprev[0:64, g, :], in_=KDa[0:64, TS:TS + 128], func=AF.Copy), [kdb], [self.kprevb])
                S.op("act", lambda: nc.scalar.activation(out=self.KTprev[64:128, g, :], in_=KDb[64:128, TS:TS + 128], func=AF.Copy), [kdb], [self.kprevb])
                for n in range(NT):
                    first = (st == 0 and n == 0)
                    mask = self.swa_mask[:, 0, :] if first else self.swa_mask[:, 1, :]

                    def chain(hh, lane):
                        head = 8 * g + hh
                        mi, half = hh // 2, hh % 2
                        KDh = KDa if half == 0 else KDb
                        pS, pSb = lanes[lane]["pS"]
                        pK, pKb = lanes[lane]["pK"]
                        ko = lanes[lane]["ko"]
                        pA, pAb = lanes[lane]["pA"]
                        smx_, smxb_ = lanes[lane]["smx"]
                        pr_, prb_ = lanes[lane]["pr"]
                        pT_, pTb_ = lanes[lane]["pT"]
                        sm_, smb_ = lanes[lane]["sm"]
                        S.op("pe", lambda: nc.tensor.matmul(pS[:, 0:256], lhsT=QT[:, mi, n * 128:(n + 1) * 128],
                                                            rhs=KDh[:, n * 128:n * 128 + 256], start=True, stop=True),
                             [qb[n // 4], kdb], [pSb])
                        yield
                        S.op("dve", lambda: nc.vector.scalar_tensor_tensor(out=smx_[:], in0=pS[:, 0:256], scalar=0.125, in1=mask,
                                                                           op0=ALU.mult, op1=ALU.add), [pSb], [smxb_])
                        yield
                        S.op("dve", lambda: nc.vector.reduce_max(out=sm_[:, 0:1], in_=smx_[:], axis=AX.X), [smxb_], [smb_])
                        yield
                        S.op("dve", lambda: nc.vector.tensor_scalar(out=sm_[:, 1:2], in0=sm_[:, 0:1], scalar1=self.sinks[:, head:head + 1],
                                                                    scalar2=-1.0, op0=ALU.max, op1=ALU.mult), [smb_], [smb_])
                        yield
                        S.op("act", lambda: nc.scalar.activation(out=pr_[:], in_=smx_[:], func=AF.Exp, bias=sm_[:, 1:2], scale=1.0,
                                                                 accum_out=sm_[:, 2:3]), [smxb_, smb_], [prb_, smb_])
                        yield
                        S.op("act", lambda: nc.scalar.activation(out=sm_[:, 3:4], in_=self.sinks[:, head:head + 1], func=AF.Exp,
                                                                 bias=sm_[:, 1:2], scale=1.0), [smb_], [smb_])

                        def fn():
                            nc.tensor.transpose(pK[:, ko:ko + 128], pr_[:, 0:128], self.ident[:])
                            return nc.tensor.transpose(pK[:, ko + 128:ko + 256], pr_[:, 128:256], self.ident[:])
                        S.op("pe", fn, [prb_], [pKb])
                        yield
                        S.op("dve", lambda: nc.vector.tensor_tensor(out=sm_[:, 4:5], in0=sm_[:, 2:3], in1=sm_[:, 3:4], op=ALU.add), [smb_], [smb_])
                        self.evac_copy(pT_[:].rearrange("p a b -> p (a b)"), pK[:, ko:ko + 256], [pKb], [pTb_])
                        yield
                        S.op("dve", lambda: nc.vector.reciprocal(out=sm_[:, 5:6], in_=sm_[:, 4:5]), [smb_], [smb_])

                        def fn():
                            nc.tensor.matmul(pA[:, 256:320], lhsT=pT_[:, 0, :], rhs=VV[:, n, g * 64:(g + 1) * 64], start=True, stop=False)
                            return nc.tensor.matmul(pA[:, 256:320], lhsT=pT_[:, 1, :], rhs=VV[:, n + 1, g * 64:(g + 1) * 64], start=False, stop=True)
                        S.op("pe", fn, [pTb_, vvb[n], vvb[n + 1]], [pAb])
                        yield
                        S.op("dve", lambda: nc.vector.tensor_scalar(out=Ot[:, hh * 64:(hh + 1) * 64], in0=pA[:, 256:320], scalar1=sm_[:, 5:6],
                                                                    scalar2=None, op0=ALU.mult), [pAb, smb_], [otbs[hh % 4]])
                        yield

                    for hp in range(2):
                        alive = [chain(4 * hp + ln, ln) for ln in range(4)]
                        while alive:
                            for gen in list(alive):
                                try:
                                    next(gen)
                                except StopIteration:
                                    alive.remove(gen)
                    self.out_transpose_store_multi(Ot, otbs, 4 * g, n * 128, hts, htb)
            S.barrier()

    def build(self):
        nc, T, TS = self.nc, self.T, self.TS
        es = self.es
        S = self.S = Sched(nc, es)
        self.xT = self.dram("xT", [16, 128, T])
        self.HT = self.dram("outT", [16, 128, T], kind="ExternalOutput")
        self.gains_d = self.dram("gains", [128, 256])
        self.w_up = self.dram("w_up", [4, D, 2 * DFF])
        self.cw_d = self.dram("conv_w", [128, 4 * 3 * 88])
        self.cb_d = self.dram("conv_b", [128, 4 * 88])
        self.w_down = self.dram("w_down", [4, DFF, D])
        self.m_w_in = self.dram("m_w_in", [2, D, 6152])
        self.m_gb_d = self.dram("m_gb", [128, 16])
        self.m_hn = self.dram("m_hn", [2, 128, 2048])
        self.m_w_out = self.dram("m_w_out", [2, D, D])
        self.s_w_qkv = self.dram("s_w_qkv", [1, D, 2560])
        self.sinks_d = self.dram("s_sinks", [128, 32])
        self.s_w_out = self.dram("s_w_out", [1, D, D])
        self.r_w_in = self.dram("r_w_in", [1, D, 12288])
        self.r_hn = self.dram("r_hn", [1, 128, 4096])
        self.r_w_out = self.dram("r_w_out", [1, 4096, D])
        cst = {n: self.dram(n, shp) for n, shp in [("c_ident", [128, 128]), ("c_ones", [128, 128]), ("c_tri", [128, 128]),
                                                    ("c_ustr", [128, 128]), ("c_mscale", [128, 128]), ("c_swamask", [128, 512]),
                                                    ("c_dmaskT", [128, 8 * 128]), ("c_idec", [128, 8]), ("c_statew", [128, 8])]}
        self.cosT_d = self.dram("c_cosT", [128, T])
        self.sinT_d = self.dram("c_sinT", [128, T])
        self.Y = [self.dram(f"Y{p}", [16, 128, TS], kind="Internal") for p in range(3)]
        self.CstD = self.dram("CstD", [8, 128, 1024], kind="Internal")
        self.HTmix = self.dram("HTmix", [32, 128, TS], BF16, kind="Internal")
        self.GT = self.dram("GT", [NFT, 128, TS], BF16, kind="Internal")
        self.ybuf = [Buf("Y0"), Buf("Y1"), Buf("Y2")]
        self.cstdb = Buf("CstD")
        self.htbuf = Buf("HT")
        self.srcbuf = Buf("src")
        lg = np.log(1.0 - np.power(2.0, -5.0 - np.arange(8, dtype=np.float64)))
        self.gamma_L = np.exp(lg * 128.0)
        sb = lambda n, s, d: self.sb(es, n, s, d)
        self.ident = sb("ident", [128, 128], BF16)
        self.ones_bf = sb("ones_bf", [128, 128], BF16)
        self.ones_f = sb("ones_f", [128, 128], F32)
        self.tri_f = sb("tri_f", [128, 128], F32)
        self.ustr_f = sb("ustr_f", [128, 128], F32)
        self.mscale = sb("mscale", [128, 128], F32)
        self.swa_mask = sb("swa_mask", [128, 2, 256], F32)
        self.dmaskT = sb("dmaskT", [128, 8, 128], F32)
        self.idec = sb("idec", [128, 8], F32)
        self.statew = sb("statew", [128, 8], F32)
        self.gains = sb("gains_sb", [128, 256], F32)
        self.cw = sb("cw_sb", [128, 4 * 3 * 88], F32)
        self.cbias = sb("cb_sb", [128, 4 * 88], F32)
        self.m_gb = sb("m_gb_sb", [128, 16], F32)
        self.sinks = sb("sinks_sb", [128, 32], F32)
        self.rhs3 = sb("rhsbuf", [128, 16, TS], BF16)
        self.rb = [Buf(f"rhs{b}") for b in range(TS // 256)]
        self.wslots = [sb(f"wslot{i}", [128, 16, 512], BF16) for i in range(2)]
        self.wbuf = [[Buf("w0a"), Buf("w0b")], [Buf("w1a"), Buf("w1b")]]
        self.pref = None
        self.wnext = 0
        self.lnext = 0
        self.Cst = sb("Cst", [128, 1024], F32)
        self.cstb = Buf("Cst")
        self.nst = sb("nst", [128, 8], F32)
        self.nstb = Buf("nst")
        self.KTprev = sb("KTprev", [128, 4, 128], BF16)
        self.kprevb = Buf("kprev")
        self.Vprev = sb("Vprev", [128, 256], BF16)
        self.vprevb = Buf("vprev")
        self.halo = sb("halo", [128, 2, NFT, 2], F32)
        self.halob = Buf("halo")
        ps = lambda n, dt, w: es.enter_context(nc.psum_tensor(n, [128, w], dt))
        self.lbank = [(ps(f"pl{i}", F32, 512), Buf(f"pl{i}")) for i in range(4)]
        self.pS = self.lbank[0]
        self.pA = self.lbank[1]
        self.pB = self.lbank[2]
        self.pmisc = self.lbank[3]
        self.pT = [(ps("pt0", BF16, 1024), Buf("pt0")), (ps("pt1", BF16, 1024), Buf("pt1"))]
        cb = Buf("consts")
        for dst, src in [(self.ident, "c_ident"), (self.ones_bf, "c_ones")]:
            S.dma("pool", dst[:], cst[src][:, :], [], [cb])
        for dst, src in [(self.ones_f, "c_ones"), (self.tri_f, "c_tri"), (self.ustr_f, "c_ustr"), (self.mscale, "c_mscale"),
                         (self.idec, "c_idec"), (self.statew, "c_statew")]:
            S.dma("sp", dst[:], cst[src][:, :], [], [cb])
        S.dma("sp", self.swa_mask[:].rearrange("p a b -> p (a b)"), cst["c_swamask"][:, :], [], [cb])
        S.dma("sp", self.dmaskT[:].rearrange("p a b -> p (a b)"), cst["c_dmaskT"][:, :], [], [cb])
        S.dma("sp", self.gains[:], self.gains_d[:, :], [], [cb])
        S.dma("sp", self.cw[:], self.cw_d[:, :], [], [cb])
        S.dma("sp", self.cbias[:], self.cb_d[:, :], [], [cb])
        S.dma("sp", self.m_gb[:], self.m_gb_d[:, :], [], [cb])
        S.dma("sp", self.sinks[:], self.sinks_d[:, :], [], [cb])
        for k in range(16):
            S.dma("sp", self.HT[k, :, :], self.xT[k, :, :], [], [self.htbuf])
        S.barrier()
        for l in self.layers:
            kind, j = l % 3, l // 3
            S.op("pool", lambda: nc.gpsimd.memset(self.Cst[:], 0.0), [], [self.cstb])
            for hh in range(8):
                S.dma("sp", self.CstD[hh, :, :], self.Cst[:], [self.cstb], [self.cstdb])
            S.op("pool", lambda: nc.gpsimd.memset(self.nst[:], 0.0), [], [self.nstb])
            S.op("pool", lambda: nc.gpsimd.memset(self.KTprev[:], 0.0), [], [self.kprevb])
            S.op("pool", lambda: nc.gpsimd.memset(self.Vprev[:], 0.0), [], [self.vprevb])
            S.op("pool", lambda: nc.gpsimd.memset(self.halo[:], 0.0), [], [self.halob])
            import os as _os
            mx = int(_os.environ.get("MAXSTAGE", "1000"))
            for st in range(self.NST):
                pf = None
                if kind == 1:
                    pf = (self.s_w_qkv[j], 0, 16, [(2304, 256)])
                elif kind == 2:
                    pf = (self.r_w_in[j], 0, 16, [(0, 256), (2048, 256)])
                if self.stg < mx: self.norm_stage(st, (l * 4 + 0) * 16, pf)
                self.stg += 1
                if kind == 1:
                    if self.stg < mx: self.swa_stage(st, j)
                    Wo, Kt = self.s_w_out[j], 16
                else:
                    if self.stg < mx: self.linattn_stage(st, kind, j)
                    Wo, Kt = (self.m_w_out[j], 16) if kind == 0 else (self.r_w_out[j], 32)
                self.stg += 1
                if self.stg < mx: parts = self.proj_out_stage(Wo, Kt, self.HTmix)
                self.stg += 1
                if self.stg < mx: self.finalize_stage(st, parts, (l * 4 + 1) * 16)
                self.stg += 1
                if self.stg < mx: self.norm_stage(st, (l * 4 + 2) * 16, (self.w_up[l], 0, 16, [(0, 256), (DFF, 256)]))
                self.stg += 1
                if self.stg < mx: self.ffn_up_stage(st, l)
                self.stg += 1
                if self.stg < mx: parts = self.proj_out_stage(self.w_down[l], NFT, self.GT)
                self.stg += 1
                if self.stg < mx: self.finalize_stage(st, parts, (l * 4 + 3) * 16)
                self.stg += 1
        S.barrier()
        self.es.close()
        return nc


def host_consts(T):
    c = {}
    i = np.arange(128)
    c["c_ident"] = np.eye(128, dtype=np.float32)
    c["c_ones"] = np.ones((128, 128), np.float32)
    c["c_tri"] = (i[:, None] <= i[None, :]).astype(np.float32)
    c["c_ustr"] = (i[:, None] > i[None, :]).astype(np.float32)
    c["c_mscale"] = (i[:, None] <= i[None, :]).astype(np.float32) * np.float32(256 ** -0.5)
    q = i[:, None]
    jj = np.arange(256)[None, :]
    valid = (jj > q) & (jj <= q + 128)
    m1 = np.where(valid, 0.0, -30000.0).astype(np.float32)
    m0 = np.where(valid & (jj >= 128), 0.0, -30000.0).astype(np.float32)
    c["c_swamask"] = np.concatenate([m0, m1], axis=1)
    lg = np.log(1.0 - np.power(2.0, -5.0 - np.arange(8, dtype=np.float64)))
    rel = (i[None, :] - i[:, None]).astype(np.float64)
    dm = np.where(rel[None] >= 0, np.exp(lg[:, None, None] * np.maximum(rel[None], 0.0)), 0.0) * (256 ** -0.5)
    c["c_dmaskT"] = np.ascontiguousarray(dm.transpose(1, 0, 2).reshape(128, 8 * 128)).astype(np.float32)
    c["c_idec"] = np.exp(lg[None, :] * (i[:, None] + 1.0)).astype(np.float32)
    c["c_statew"] = (np.exp(lg[None, :] * (127.0 - i[:, None])) * (256 ** -0.5)).astype(np.float32)
    half = 128
    inv = np.power(np.float32(10000.0), -np.arange(half, dtype=np.float32) / np.float32(half)).astype(np.float32)
    pos = np.arange(T, dtype=np.float32)
    ang = (pos[None, :] * inv[:, None]).astype(np.float32)
    c["c_cosT"] = np.cos(ang).astype(np.float32)
    c["c_sinT"] = np.sin(ang).astype(np.float32)
    return c


def host_layout(inp):
    o = {}
    g = inp["norm_gains"].reshape(4, 4, 16, 128)
    o["gains"] = np.ascontiguousarray(g.transpose(3, 0, 1, 2).reshape(128, 256))
    cw = inp["ffn_conv_w"].reshape(4, 3, 88, 128)
    o["conv_w"] = np.ascontiguousarray(cw.transpose(3, 0, 1, 2).reshape(128, 4 * 3 * 88))
    cbv = inp["ffn_conv_b"].reshape(4, 88, 128)
    o["conv_b"] = np.ascontiguousarray(cbv.transpose(2, 0, 1).reshape(128, 4 * 88))
    o["w_up"] = inp["ffn_w_up"]
    o["w_down"] = inp["ffn_w_down"]
    o["m_w_in"] = inp["mlstm_w_in"]
    gb = inp["mlstm_gate_b"].reshape(2, 8)
    o["m_gb"] = np.ascontiguousarray(np.broadcast_to(gb.reshape(1, 16), (128, 16)))
    o["m_hn"] = np.ascontiguousarray(np.broadcast_to(inp["mlstm_head_norm"][:, None, :], (2, 128, 2048)))
    o["m_w_out"] = inp["mlstm_w_out"]
    o["s_w_qkv"] = inp["swa_w_qkv"]
    o["s_sinks"] = np.ascontiguousarray(np.broadcast_to(inp["swa_sinks"].reshape(1, 32), (128, 32)))
    o["s_w_out"] = inp["swa_w_out"]
    o["r_w_in"] = inp["ret_w_in"]
    o["r_hn"] = np.ascontiguousarray(np.broadcast_to(inp["ret_head_norm"][:, None, :], (1, 128, 4096)))
    o["r_w_out"] = inp["ret_w_out"]
    return {k: np.ascontiguousarray(v, dtype=np.float32) for k, v in o.items()}


def run(inp, T, TS, layers, ncores):
    x = inp["x"]
    b = Builder(T, TS, layers)
    nc = b.build()
    shared = host_layout(inp)
    shared.update(host_consts(T))
    in_maps = []
    for c in range(ncores):
        m = dict(shared)
        xc = x[c]
        m["xT"] = np.ascontiguousarray(xc.T.reshape(16, 128, T))
        in_maps.append(m)
    res = run_bass_kernel_spmd(nc, in_maps, core_ids=list(range(ncores)))
    outs = []
    for c in range(ncores):
        oT = res.results[c]["outT"]
        outs.append(np.ascontiguousarray(oT.reshape(2048, T).T))
    return np.stack(outs, axis=0).astype(np.float32)


def kernel(**inputs):
    inp = {k: np.asarray(v) for k, v in inputs.items()}
    return run(inp, 4096, 2048, [0, 1, 2, 3], 4)
```

```python
import math
from contextlib import ExitStack
import numpy as np
import concourse.bass as bass
import concourse.mybir as mybir
from concourse.bass_utils import run_bass_kernel_spmd

F32 = mybir.dt.float32
BF16 = mybir.dt.bfloat16
AF = mybir.ActivationFunctionType
ALU = mybir.AluOpType
AX = mybir.AxisListType

D = 2048
DFF = 5632
NFT = DFF // 128
EPS = 1e-6
NSD = 12


class Buf:
    __slots__ = ("name", "w", "r")

    def __init__(self, name):
        self.name = name
        self.w = None
        self.r = []


class Sched:
    ENG = ["pe", "act", "dve", "pool", "sp"]

    def __init__(self, nc, es):
        self.nc = nc
        self.h = {"pe": nc.tensor, "act": nc.scalar, "dve": nc.vector, "pool": nc.gpsimd, "sp": nc.sync}
        self.sem = {e: es.enter_context(nc.semaphore("c_" + e)) for e in self.ENG}
        self.cnt = {e: 0 for e in self.ENG}
        self.dsem = {e: [es.enter_context(nc.semaphore(f"d_{e}{i}")) for i in range(NSD)] for e in ("sp", "pool")}
        self.ndma = {"sp": 0, "pool": 0}
        self.wc = {e: {} for e in self.ENG}
        self.wd = {e: set() for e in self.ENG}

    def _wait(self, eng, tok):
        kind, e2, v = tok
        E = self.h[eng]
        if kind == "c":
            if e2 == eng and eng == "pe":
                return
            if self.wc[eng].get(e2, 0) >= v:
                return
            self.wc[eng][e2] = v
            E.wait_ge(self.sem[e2], v)
        else:
            if tok in self.wd[eng]:
                return
            self.wd[eng].add(tok)
            E.wait_ge(self.dsem[e2][v % NSD], 16 * (v // NSD + 1))

    def _deps(self, eng, reads, writes):
        deps = []
        for b in reads:
            if b.w is not None:
                deps.append(b.w)
        for b in writes:
            if b.w is not None:
                deps.append(b.w)
            deps.extend(b.r)
        best = {}
        for d in deps:
            if d[0] == "c":
                if best.get(d[1], 0) < d[2]:
                    best[d[1]] = d[2]
            else:
                self._wait(eng, d)
        for e2, v in best.items():
            self._wait(eng, ("c", e2, v))

    def _mark(self, tok, reads, writes):
        for b in reads:
            b.r.append(tok)
        for b in writes:
            b.w = tok
            b.r = []

    def op(self, eng, fn, reads=(), writes=()):
        self._deps(eng, reads, writes)
        ins = fn()
        self.cnt[eng] += 1
        ins.then_inc(self.sem[eng], 1)
        tok = ("c", eng, self.cnt[eng])
        self._mark(tok, reads, writes)
        return tok

    def dma(self, eng, out, in_, reads=(), writes=()):
        k = self.ndma[eng]
        self.ndma[eng] += 1
        if k >= NSD:
            self._wait(eng, ("d", eng, k - NSD))
        self._deps(eng, reads, writes)
        ins = self.h[eng].dma_start(out=out, in_=in_)
        ins.then_inc(self.dsem[eng][k % NSD], 16)
        tok = ("d", eng, k)
        self._mark(tok, reads, writes)
        return tok

    def barrier(self):
        toks = []
        for e in self.ENG:
            if self.cnt[e] > 0:
                toks.append(("c", e, self.cnt[e]))
        for e in ("sp", "pool"):
            for k in range(max(0, self.ndma[e] - NSD), self.ndma[e]):
                toks.append(("d", e, k))
        for e in self.ENG:
            for t in toks:
                if t[0] == "c" and t[1] == e:
                    if self.wc[e].get(e, 0) < t[2]:
                        self.wc[e][e] = t[2]
                        self.h[e].wait_ge(self.sem[e], t[2])
                    continue
                self._wait(e, t)


class Builder:
    def __init__(self, T, TS, layers):
        self.T, self.TS, self.layers = T, TS, layers
        self.NT = TS // 128
        self.NB = TS // 512
        self.NST = T // TS
        self.BN = 256
        self.nc = bass.Bass("TRN2", target_bir_lowering=False)
        self.es = ExitStack()
        self.evk = 0
        self.stg = 0

    def dram(self, name, shape, dt=F32, kind="ExternalInput"):
        return self.nc.dram_tensor(name, list(shape), dt, kind=kind).ap()

    def psl(self, es, name, dt, w):
        self.uid = getattr(self, "uid", 0) + 1
        return es.enter_context(self.nc.psum_tensor(f"{name}_{self.uid}", [128, w], dt))

    def sb(self, es, name, shape, dt):
        self.uid = getattr(self, "uid", 0) + 1
        return es.enter_context(self.nc.sbuf_tensor(f"{name}_{self.uid}", list(shape), dt))

    def evac_copy(self, out, in_, reads, writes):
        self.evk += 1
        if self.evk % 2 == 0:
            self.S.op("act", lambda: self.nc.scalar.activation(out=out, in_=in_, func=AF.Copy), reads, writes)
        else:
            self.S.op("dve", lambda: self.nc.vector.tensor_copy(out=out, in_=in_), reads, writes)

    def load_w(self, slot, W, row0, KT, segs):
        wt = self.wslots[slot]
        off = 0
        for i, (c0, n) in enumerate(segs):
            src = W[row0:row0 + KT * 128, c0:c0 + n].rearrange("(kt p) c -> p kt c", p=128)
            self.S.dma("pool", wt[:, 0:KT, off:off + n], src, reads=(), writes=(self.wbuf[slot][i],))
            off += n
        return off

    def get_w(self, W, row0, KT, segs):
        key = (id(W.tensor) if hasattr(W, "tensor") else 0, str(W), row0, KT, tuple(segs))
        if self.pref is not None and self.pref[0] == key:
            slot, ncols = self.pref[1], self.pref[2]
            self.pref = None
            return slot, ncols
        assert self.pref is None, "unused prefetch"
        slot = self.wnext % 2
        self.wnext += 1
        return slot, self.load_w(slot, W, row0, KT, segs)

    def prefetch_w(self, W, row0, KT, segs):
        key = (id(W.tensor) if hasattr(W, "tensor") else 0, str(W), row0, KT, tuple(segs))
        slot = self.wnext % 2
        self.wnext += 1
        self.pref = (key, slot, self.load_w(slot, W, row0, KT, segs))

    def lin_feat(self, W, row0, KT, groups, rhs_ap, rhs_bufs, evac, block_outer=False, ncolmax=512, banks=None):
        nc, S = self.nc, self.S
        for gi, segs in enumerate(groups):
            slot, ncols = self.get_w(W, row0, KT, segs)
            wt = self.wslots[slot]
            nm = ncols // 128

            bankl = banks if banks is not None else self.lbank

            def group(mi, b, bank):
                pa, pb = bankl[bank]

                def fn():
                    ins = None
                    for kt in range(KT):
                        ins = nc.tensor.matmul(pa[:, 0:512], lhsT=wt[:, kt, mi * 128:(mi + 1) * 128],
                                               rhs=rhs_ap(kt, b), start=(kt == 0), stop=(kt == KT - 1))
                    return ins
                S.op("pe", fn, reads=self.wbuf[slot] + rhs_bufs(b), writes=[pb])
                return pa, pb

            if block_outer:
                for b in range(self.NB):
                    outs = []
                    for mi in range(nm):
                        outs.append(group(mi, b, mi % 4))
                    evac(gi, b, outs)
            else:
                for mi in range(nm):
                    for b in range(self.NB):
                        bank = self.lnext % len(bankl)
                        self.lnext += 1
                        pa, pb = group(mi, b, bank)
                        evac(gi, mi, b, pa, pb)

    def lin_tok(self, W, row0, KT, segs, evac):
        nc, S = self.nc, self.S
        slot, ncols = self.get_w(W, row0, KT, segs)
        wt = self.wslots[slot]
        for ti in range(self.NT):
            bank = self.lnext % 4
            self.lnext += 1
            pa, pb = self.lbank[bank]

            def fn():
                ins = None
                for kt in range(KT):
                    ins = nc.tensor.matmul(pa[:, 0:ncols], lhsT=self.rhs3[:, kt, ti * 128:(ti + 1) * 128],
                                           rhs=wt[:, kt, 0:ncols], start=(kt == 0), stop=(kt == KT - 1))
                return ins
            S.op("pe", fn, reads=self.wbuf[slot] + [self.rb[ti // 2]], writes=[pb])
            evac(ti, pa, pb)

    def rhs_blk(self, kt, b):
        return self.rhs3[:, kt, b * 512:(b + 1) * 512]

    def rhs_blk_bufs(self, b):
        return [self.rb[2 * b], self.rb[2 * b + 1]]

    def rstd_from_ss(self, es_tiles, ss_ap, ss_buf, rs, rsb, n):
        nc, S = self.nc, self.S
        S.op("dve", lambda: nc.vector.tensor_scalar(out=rs, in0=ss_ap, scalar1=1.0 / D, scalar2=EPS,
                                                    op0=ALU.mult, op1=ALU.add), [ss_buf], [rsb])
        S.op("act", lambda: nc.scalar.activation(out=rs, in_=rs, func=AF.Sqrt), [rsb], [rsb])
        S.op("dve", lambda: nc.vector.reciprocal(out=rs, in_=rs), [rsb], [rsb])

    def norm_stage(self, st, gcol, pref=None):
        nc, S, BN = self.nc, self.S, self.BN
        if pref is not None:
            self.prefetch_w(*pref)
        with ExitStack() as es:
            xin = [self.sb(es, f"n_xin{i}", [128, 16, BN], F32) for i in range(2)]
            sqb = [self.sb(es, f"n_sqb{i}", [128, 16, BN], BF16) for i in range(2)]
            rs = [self.sb(es, f"n_rs{i}", [128, BN], F32) for i in range(2)]
            xb = [Buf("xin"), Buf("xin")]
            qb = [Buf("sq"), Buf("sq")]
            rsb = [Buf("rs"), Buf("rs")]
            for b in range(self.TS // BN):
                s = b % 2
                c0 = st * self.TS + b * BN
                S.dma("sp", xin[s][:], self.HT[:, :, c0:c0 + BN].rearrange("k p t -> p k t"), [], [xb[s]])
                S.op("act", lambda: nc.scalar.activation(out=sqb[s][:], in_=xin[s][:], func=AF.Square), [xb[s]], [qb[s]])
                pa, pb = self.lbank[b % 4]

                def fn():
                    ins = None
                    for kt in range(16):
                        ins = nc.tensor.matmul(pa[:, 0:BN], lhsT=self.ones_bf[:], rhs=sqb[s][:, kt, :],
                                               start=(kt == 0), stop=(kt == 15))
                    return ins
                S.op("pe", fn, [qb[s]], [pb])
                self.rstd_from_ss(None, pa[:, 0:BN], pb, rs[s][:], rsb[s], BN)
                for kt in range(16):
                    S.op("dve", lambda: nc.vector.scalar_tensor_tensor(
                        out=self.rhs3[:, kt, b * BN:(b + 1) * BN], in0=xin[s][:, kt, :],
                        scalar=self.gains[:, gcol + kt:gcol + kt + 1], in1=rs[s][:],
                        op0=ALU.mult, op1=ALU.mult), [xb[s], rsb[s]], [self.rb[b]])
            S.barrier()

    def finalize_stage(self, st, parts, gcol):
        nc, S, BN = self.nc, self.S, 128
        with ExitStack() as es:
            ys = [[self.sb(es, f"f_y{p}_{i}", [128, 16, BN], F32) for i in range(2)] for p in range(len(parts))]
            hx = [self.sb(es, f"f_hx{i}", [128, 16, BN], F32) for i in range(2)]
            sqb = [self.sb(es, f"f_sqb{i}", [128, 16, BN], BF16) for i in range(2)]
            rs = [self.sb(es, f"f_rs{i}", [128, BN], F32) for i in range(2)]
            yb = [[Buf("y") for i in range(2)] for p in parts]
            hb = [Buf("hx"), Buf("hx")]
            qb = [Buf("sq"), Buf("sq")]
            rsb = [Buf("rs"), Buf("rs")]
            def loads(b):
                s = b % 2
                c0 = st * self.TS + b * BN
                for p, Y in enumerate(parts):
                    S.dma("sp", ys[p][s][:], Y[:, :, b * BN:(b + 1) * BN].rearrange("k p t -> p k t"),
                          [self.ybuf[p]], [yb[p][s]])
                S.dma("sp", hx[s][:], self.HT[:, :, c0:c0 + BN].rearrange("k p t -> p k t"), [self.htbuf], [hb[s]])
            nblk = self.TS // BN
            loads(0)
            for b in range(nblk):
                s = b % 2
                c0 = st * self.TS + b * BN
                if b + 1 < nblk:
                    loads(b + 1)
                y = ys[0][s]
                for p in range(1, len(parts)):
                    if p == 1:
                        S.op("dve", lambda: nc.vector.tensor_tensor(out=y[:], in0=y[:], in1=ys[p][s][:], op=ALU.add),
                             [yb[0][s], yb[p][s]], [yb[0][s]])
                    else:
                        S.op("pool", lambda: nc.gpsimd.tensor_tensor(out=y[:], in0=y[:], in1=ys[p][s][:], op=ALU.add),
                             [yb[0][s], yb[p][s]], [yb[0][s]])
                S.op("act", lambda: nc.scalar.activation(out=sqb[s][:], in_=y[:], func=AF.Square), [yb[0][s]], [qb[s]])
                pa, pb = self.lbank[b % 4]

                def fn():
                    ins = None
                    for kt in range(16):
                        ins = nc.tensor.matmul(pa[:, 0:BN], lhsT=self.ones_bf[:], rhs=sqb[s][:, kt, :],
                                               start=(kt == 0), stop=(kt == 15))
                    return ins
                S.op("pe", fn, [qb[s]], [pb])
                self.rstd_from_ss(None, pa[:, 0:BN], pb, rs[s][:], rsb[s], BN)
                for kt in range(16):
                    S.op("dve", lambda: nc.vector.scalar_tensor_tensor(
                        out=y[:, kt, :], in0=y[:, kt, :], scalar=self.gains[:, gcol + kt:gcol + kt + 1],
                        in1=rs[s][:], op0=ALU.mult, op1=ALU.mult), [yb[0][s], rsb[s]], [yb[0][s]])
                S.op("pool", lambda: nc.gpsimd.tensor_tensor(out=hx[s][:], in0=hx[s][:], in1=y[:], op=ALU.add),
                     [yb[0][s], hb[s]], [hb[s]])
                S.dma("sp", self.HT[:, :, c0:c0 + BN].rearrange("k p t -> p k t"), hx[s][:], [hb[s]], [self.htbuf])
            S.barrier()

    def proj_out_stage(self, W, Ktiles, src, gt_mode=False):
        nc, S = self.nc, self.S
        splits = [16] * (Ktiles // 16) + ([Ktiles % 16] if Ktiles % 16 else [])
        nparts = len(splits)
        gcols = 512
        with ExitStack() as es:
            ysb = [self.sb(es, f"o_y{i}", [128, 512], F32) for i in range(2)]
            yb = [Buf("ysb"), Buf("ysb")]
            rbufs = [(self.rhs3, self.rb)]
            if nparts > 1:
                rbufs.append((self.sb(es, "rhsB", [128, 16, self.TS], BF16), [Buf(f"rhsB{b}") for b in range(self.TS // 256)]))
            k = [0]

            def loads(p):
                buf, bufs = rbufs[p % len(rbufs)]
                for kt in range(splits[p]):
                    S.dma("sp", buf[:, kt, :], src[16 * p + kt, :, :], [self.srcbuf], bufs)
            loads(0)
            for p in range(nparts):
                KT = splits[p]
                k0 = 16 * p
                if p + 1 < nparts:
                    loads(p + 1)
                buf, bufs = rbufs[p % len(rbufs)]
                Y = self.Y[p]

                def evac(gi, mi, b, pa, pb):
                    s = k[0] % 2
                    k[0] += 1
                    self.evac_copy(ysb[s][:], pa[:, 0:512], [pb], [yb[s]])
                    dt = gi * (gcols // 128) + mi
                    S.dma("sp", Y[dt, :, b * 512:(b + 1) * 512], ysb[s][:], [yb[s]], [self.ybuf[p]])
                groups = [[(c, gcols)] for c in range(0, D, gcols)]
                self.lin_feat(W, k0 * 128, KT, groups, lambda kt, b: buf[:, kt, b * 512:(b + 1) * 512],
                              lambda b: [bufs[2 * b], bufs[2 * b + 1]], evac)
            S.barrier()
        return [self.Y[p] for p in range(nparts)]

    def ffn_up_stage(self, st, l):
        nc, S, TS = self.nc, self.S, self.TS
        W = self.w_up[l]
        with ExitStack() as es:
            UU = [[self.sb(es, f"u_{s}_{m}", [128, TS + 2], F32) for m in range(2)] for s in range(2)]
            ub = [[Buf("uu") for m in range(2)] for s in range(2)]
            cc = [[self.sb(es, f"c_{s}_{m}", [128, TS], F32) for m in range(2)] for s in range(2)]
            cb = [[Buf("cc") for m in range(2)] for s in range(2)]
            gt = [self.sb(es, f"gt_{s}", [128, TS], BF16) for s in range(2)]
            gb = [Buf("gt"), Buf("gt")]
            cwb = self.cw
            banks = list(self.lbank) + [(self.psl(es, "pf0", F32, 512), Buf("pf0")), (self.psl(es, "pf1", F32, 512), Buf("pf1"))]
            state = {"slot": 0, "wt": None, "bk": 0}

            def mm_tile(j):
                g2, jj = j // 2, j % 2
                s = j % 2
                if jj == 0:
                    state["slot"], _ = self.get_w(W, 0, 16, [(g2 * 256, 256), (DFF + g2 * 256, 256)])
                slot = state["slot"]
                wt = self.wslots[slot]
                for m in range(2):
                    S.op("act", lambda: nc.scalar.activation(out=UU[s][m][:, 0:2], in_=self.halo[:, m, j, :], func=AF.Copy),
                         [self.halob], [ub[s][m]])
                for m in range(2):
                    mi = m * 2 + jj
                    for b in range(self.NB):
                        pa, pb = banks[state["bk"] % 6]
                        state["bk"] += 1

                        def fn():
                            ins = None
                            for kt in range(16):
                                ins = nc.tensor.matmul(pa[:, 0:512], lhsT=wt[:, kt, mi * 128:(mi + 1) * 128],
                                                       rhs=self.rhs_blk(kt, b), start=(kt == 0), stop=(kt == 15))
                            return ins
                        S.op("pe", fn, self.wbuf[slot] + self.rhs_blk_bufs(b), [pb])
                        self.evac_copy(UU[s][m][:, 2 + b * 512:2 + (b + 1) * 512], pa[:, 0:512], [pb], [ub[s][m]])

            def conv_tile(j):
                s = j % 2
                for m in range(2):
                    col = m * NFT + j
                    u = UU[s][m]
                    c = cc[s][m]
                    w0 = cwb[:, (l * 3 + 0) * 88 + col:(l * 3 + 0) * 88 + col + 1]
                    w1 = cwb[:, (l * 3 + 1) * 88 + col:(l * 3 + 1) * 88 + col + 1]
                    w2 = cwb[:, (l * 3 + 2) * 88 + col:(l * 3 + 2) * 88 + col + 1]
                    bia = self.cbias[:, l * 88 + col:l * 88 + col + 1]
                    S.op("act", lambda: nc.scalar.activation(out=c[:], in_=u[:, 2:TS + 2], func=AF.Identity,
                                                             bias=bia, scale=w2), [ub[s][m]], [cb[s][m]])
                    S.op("dve", lambda: nc.vector.scalar_tensor_tensor(out=c[:], in0=u[:, 1:TS + 1], scalar=w1, in1=c[:],
                                                                       op0=ALU.mult, op1=ALU.add), [ub[s][m], cb[s][m]], [cb[s][m]])
                    S.op("dve", lambda: nc.vector.scalar_tensor_tensor(out=c[:], in0=u[:, 0:TS], scalar=w0, in1=c[:],
                                                                       op0=ALU.mult, op1=ALU.add), [ub[s][m], cb[s][m]], [cb[s][m]])
                    S.op("act", lambda: nc.scalar.activation(out=self.halo[:, m, j, :], in_=u[:, TS:TS + 2], func=AF.Copy),
                         [ub[s][m]], [self.halob])
                S.op("act", lambda: nc.scalar.activation(out=cc[s][0][:], in_=cc[s][0][:], func=AF.Silu),
                     [cb[s][0]], [cb[s][0]])
                S.op("dve", lambda: nc.vector.tensor_tensor(out=gt[s][:], in0=cc[s][0][:], in1=cc[s][1][:], op=ALU.mult),
                     [cb[s][0], cb[s][1]], [gb[s]])
                S.dma("sp", self.GT[j, :, :], gt[s][:], [gb[s]], [self.srcbuf])

            for j in range(NFT + 1):
                if j < NFT:
                    mm_tile(j)
                if j >= 1:
                    conv_tile(j - 1)
            S.barrier()

    def out_transpose_store(self, ho, hob, c0tile, tok0, hts, htb):
        nc, S = self.nc, self.S
        pa, pb = self.pT[1]

        def fn():
            ins = None
            for c in range(4):
                ins = nc.tensor.transpose(pa[:, c * 128:(c + 1) * 128], ho[:, c * 128:(c + 1) * 128], self.ident[:])
            return ins
        S.op("pe", fn, [hob], [pb])
        S.op("act", lambda: nc.scalar.activation(out=hts[:], in_=pa[:, 0:512], func=AF.Copy), [pb], [htb])
        S.dma("sp", self.HTmix[c0tile:c0tile + 4, :, tok0:tok0 + 128].rearrange("c p t -> p c t"),
              hts[:].rearrange("p (c t) -> p c t", c=4), [htb], [self.srcbuf])

    def out_transpose_store_multi(self, ho, hobs, c0tile, tok0, hts, htb):
        nc, S = self.nc, self.S
        pa, pb = self.pT[1]

        def fn():
            ins = None
            for c in range(4):
                ins = nc.tensor.transpose(pa[:, c * 128:(c + 1) * 128], ho[:, c * 128:(c + 1) * 128], self.ident[:])
            return ins
        S.op("pe", fn, list(hobs), [pb])
        S.op("act", lambda: nc.scalar.activation(out=hts[:], in_=pa[:, 0:512], func=AF.Copy), [pb], [htb])
        S.dma("sp", self.HTmix[c0tile:c0tile + 4, :, tok0:tok0 + 128].rearrange("c p t -> p c t"),
              hts[:].rearrange("p (c t) -> p c t", c=4), [htb], [self.srcbuf])

    def linattn_stage(self, st, kind, j):
        nc, S, TS, NT = self.nc, self.S, self.TS, self.NT
        ml = (kind == 0)
        H = 4 if ml else 8
        W = self.m_w_in[j] if ml else self.r_w_in[j]
        qc, kc, vc, oc = (0, 1024, 2048, 4096) if ml else (0, 2048, 4096, 8192)
        hn = self.m_hn[j] if ml else self.r_hn[j]
        with ExitStack() as es:
            pU_loc = ([self.psl(es, "pu0", F32, 512), self.psl(es, "pu1", F32, 512)], Buf("pU"))
            QT = self.sb(es, "QT", [128, 2, TS], BF16)
            KT_ = self.sb(es, "KT", [128, 2, TS], BF16)
            V = self.sb(es, "V", [128, NT, 512], BF16)
            OG = self.sb(es, "OG", [128, NT, 512], BF16)
            qkb = [Buf("qk") for _ in range(self.NB)]
            vb = [Buf("v") for _ in range(NT)]
            ob = [Buf("og") for _ in range(NT)]
            Cbf = self.sb(es, "Cbf", [128, 2, 512], BF16)
            cbfb = Buf("cbf")
            hng = self.sb(es, "hng", [128, 512], F32)
            hngb = Buf("hng")
            Sw = self.sb(es, "Sw", [128, 128], BF16); swb = Buf("sw")
            kw = self.sb(es, "kw", [128, 256], BF16); kwb = Buf("kw")
            numA = [self.sb(es, f"numA{i}", [128, 512], F32) for i in range(3)]; nab = [Buf("numA") for _ in range(3)]
            numB = [self.sb(es, f"numB{i}", [128, 512], F32) for i in range(3)]; nbb = [Buf("numB") for _ in range(3)]
            Ucp = [self.sb(es, f"Ucp{i}", [128, 2, 512], F32) for i in range(2)]; ucb = [Buf("ucp"), Buf("ucp")]
            num = self.sb(es, "num", [128, 512], F32); numb = Buf("num")
            junk = self.sb(es, "junk", [128, 512], BF16); junkb = Buf("junk")
            hnt = self.sb(es, "hnt", [128, 512], F32); hntb = Buf("hnt")
            ho = self.sb(es, "ho", [128, 512], BF16); hob = Buf("ho")
            hts = self.sb(es, "hts", [128, 512], BF16); htb = Buf("hts")
            sm = self.sb(es, "sm", [128, 16], F32); smb = Buf("sm")
            Cf = self.Cst[:].rearrange("p (a b) -> p a b", a=2)
            if ml:
                G1 = self.sb(es, "G1", [128, NT, 8], F32); g1b = Buf("g1")
                GE = self.sb(es, "GE", [128, NT, 4], F32); geb = Buf("ge")
                LOGF = self.sb(es, "LOGF", [128, NT, 4], F32); lfb = Buf("lf")
                BG = self.sb(es, "BG", [128, NT, 8], F32); bgb = Buf("bg")
                EB = self.sb(es, "EB", [128, NT, 4], F32)
                EG = self.sb(es, "EG", [128, NT, 4], F32)
                WK = self.sb(es, "WK", [128, NT, 4], F32)
                TMP = self.sb(es, "TMPg", [128, NT, 4], F32); tmpb = Buf("tmp")
                gtb = Buf("gates")
                Lm = self.sb(es, "Lm", [128, 128], F32); lmb = Buf("lm")
                Wt = self.sb(es, "Wt", [128, 128], F32); wtb = Buf("wt")
                nbf = self.sb(es, "nbf", [128, 2], BF16); nbfb = Buf("nbf")
                dsb = [self.sb(es, f"dsb{i}", [128, 8], F32) for i in range(3)]; dsbb = [Buf("dsb") for _ in range(3)]
                gbias = self.m_gb[:, j * 8:(j + 1) * 8]

                def gev(ti, pa, pb):
                    S.op("dve", lambda: nc.vector.tensor_tensor(out=G1[:, ti, :], in0=pa[:, 0:8], in1=gbias, op=ALU.add),
                         [pb], [g1b])
                self.lin_tok(W, 0, 16, [(6144, 8)], gev)
                S.op("act", lambda: nc.scalar.activation(out=G1[:], in_=G1[:], func=AF.Tanh, scale=1.0 / 15.0), [g1b], [g1b])
                S.op("dve", lambda: nc.vector.tensor_scalar(out=G1[:], in0=G1[:], scalar1=15.0, scalar2=None, op0=ALU.mult),
                     [g1b], [g1b])
                S.op("act", lambda: nc.scalar.activation(out=GE[:], in_=G1[:, :, 4:8], func=AF.Exp, scale=-1.0), [g1b], [geb])
                S.op("act", lambda: nc.scalar.activation(out=GE[:], in_=GE[:], func=AF.Ln, bias=1.0, scale=1.0), [geb], [geb])
                S.op("dve", lambda: nc.vector.tensor_scalar(out=LOGF[:], in0=GE[:], scalar1=-1.0, scalar2=None, op0=ALU.mult),
                     [geb], [lfb])
                pa, pb = self.pmisc

                def fn():
                    ins = None
                    for ti in range(NT):
                        nc.tensor.matmul(pa[:, ti * 8:ti * 8 + 4], lhsT=self.tri_f[:], rhs=LOGF[:, ti, :], start=True, stop=True)
                        ins = nc.tensor.matmul(pa[:, ti * 8 + 4:ti * 8 + 8], lhsT=self.ones_f[:], rhs=LOGF[:, ti, :],
                                               start=True, stop=True)
                    return ins
                S.op("pe", fn, [lfb], [pb])
                S.op("act", lambda: nc.scalar.activation(out=BG[:].rearrange("p a b -> p (a b)"), in_=pa[:, 0:NT * 8], func=AF.Copy),
                     [pb], [bgb])
                S.op("act", lambda: nc.scalar.activation(out=EB[:], in_=BG[:, :, 0:4], func=AF.Exp, bias=float(math.log(1.0 / 16.0)), scale=1.0), [bgb], [gtb])
                S.op("act", lambda: nc.scalar.activation(out=EG[:], in_=BG[:, :, 4:8], func=AF.Exp), [bgb], [gtb])
                S.op("dve", lambda: nc.vector.tensor_tensor(out=TMP[:], in0=BG[:, :, 4:8], in1=BG[:, :, 0:4], op=ALU.subtract),
                     [bgb], [tmpb])
                S.op("dve", lambda: nc.vector.tensor_tensor(out=TMP[:], in0=TMP[:], in1=G1[:, :, 0:4], op=ALU.add),
                     [tmpb, g1b], [tmpb])
                S.op("act", lambda: nc.scalar.activation(out=WK[:], in_=TMP[:], func=AF.Exp), [tmpb], [gtb])
            else:
                cosT = self.sb(es, "cosT", [128, 512], F32)
                sinT = self.sb(es, "sinT", [128, 512], F32)
                tabb = Buf("tab")
                t1 = self.sb(es, "rt1", [128, 512], F32); t1b = Buf("t1")
                t2 = self.sb(es, "rt2", [128, 512], F32); t2b = Buf("t2")

            for h in range(H):
                if ml:
                    def qk_ev(gi, mi, b, pa, pb):
                        dst = QT if mi < 2 else KT_
                        self.evac_copy(dst[:, mi % 2, b * 512:(b + 1) * 512], pa[:, 0:512], [pb], [qkb[b]])
                    self.lin_feat(W, 0, 16, [[(qc + h * 256, 256), (kc + h * 256, 256)]], self.rhs_blk, self.rhs_blk_bufs, qk_ev)
                else:
                    def qk_evb(gi, b, outs):
                        S.dma("sp", cosT[:], self.cosT_d[:, st * TS + b * 512:st * TS + (b + 1) * 512], [], [tabb])
                        S.dma("sp", sinT[:], self.sinT_d[:, st * TS + b * 512:st * TS + (b + 1) * 512], [], [tabb])
                        cs = cosT[:]
                        sn = sinT[:]
                        for qi, dst in ((0, QT), (1, KT_)):
                            (p0, b0), (p1, b1) = outs[2 * qi], outs[2 * qi + 1]
                            S.op("dve", lambda: nc.vector.tensor_tensor(out=t1[:], in0=p0[:, 0:512], in1=cs, op=ALU.mult), [b0, tabb], [t1b])
                            S.op("dve", lambda: nc.vector.tensor_tensor(out=t2[:], in0=p1[:, 0:512], in1=sn, op=ALU.mult), [b1, tabb], [t2b])
                            S.op("pool", lambda: nc.gpsimd.tensor_tensor(out=dst[:, 0, b * 512:(b + 1) * 512], in0=t1[:], in1=t2[:], op=ALU.subtract),
                                 [t1b, t2b], [qkb[b]])
                            S.op("dve", lambda: nc.vector.tensor_tensor(out=t1[:], in0=p0[:, 0:512], in1=sn, op=ALU.mult), [b0, tabb], [t1b])
                            S.op("dve", lambda: nc.vector.tensor_tensor(out=t2[:], in0=p1[:, 0:512], in1=cs, op=ALU.mult), [b1, tabb], [t2b])
                            S.op("pool", lambda: nc.gpsimd.tensor_tensor(out=dst[:, 1, b * 512:(b + 1) * 512], in0=t1[:], in1=t2[:], op=ALU.add),
                                 [t1b, t2b], [qkb[b]])
                    self.lin_feat(W, 0, 16, [[(qc + h * 256, 256), (kc + h * 256, 256)]], self.rhs_blk, self.rhs_blk_bufs,
                                  qk_evb, block_outer=True)

                def v_ev(ti, pa, pb):
                    self.evac_copy(V[:, ti, :], pa[:, 0:512], [pb], [vb[ti]])
                self.lin_tok(W, 0, 16, [(vc + h * 512, 512)], v_ev)
                gfun = AF.Sigmoid if ml else AF.Silu

                def o_ev(ti, pa, pb):
                    S.op("act", lambda: nc.scalar.activation(out=OG[:, ti, :], in_=pa[:, 0:512], func=gfun), [pb], [ob[ti]])
                self.lin_tok(W, 0, 16, [(oc + h * 512, 512)], o_ev)
                if h + 1 < H:
                    self.prefetch_w(W, 0, 16, [(qc + (h + 1) * 256, 256), (kc + (h + 1) * 256, 256)])
                Cf = self.Cst[:].rearrange("p (a b) -> p a b", a=2)
                S.dma("sp", self.Cst[:], self.CstD[h, :, :], [self.cstdb], [self.cstb])
                S.dma("sp", hng[:], hn[:, h * 512:(h + 1) * 512], [], [hngb])
                S.op("act", lambda: nc.scalar.activation(out=Cbf[:], in_=Cf, func=AF.Copy), [self.cstb], [cbfb])
                if ml:
                    S.op("act", lambda: nc.scalar.activation(out=nbf[:], in_=self.nst[:, h * 2:h * 2 + 2], func=AF.Copy),
                         [self.nstb], [nbfb])
                pS, pSb = self.pS
                pm, pmb = self.pmisc
                pA, pAb = self.pA
                pB, pBb = self.pB
                pK, pKb = self.pT[0]
                pU, pUb = pU_loc
                gsl = hng[:]

                def part_F(ti):
                    p = ti % 2
                    p3 = ti % 3
                    tok = slice(ti * 128, (ti + 1) * 128)
                    qb_ = qkb[ti // 4]

                    def fn():
                        nc.tensor.matmul(pS[:, 0:128], lhsT=KT_[:, 0, tok], rhs=QT[:, 0, tok], start=True, stop=False)
                        return nc.tensor.matmul(pS[:, 0:128], lhsT=KT_[:, 1, tok], rhs=QT[:, 1, tok], start=False, stop=True)
                    S.op("pe", fn, [qb_], [pSb])
                    if ml:
                        S.op("pool", lambda: nc.gpsimd.tensor_scalar(out=Lm[:], in0=self.tri_f[:], scalar1=LOGF[:, ti, h:h + 1],
                                                                     scalar2=1.0, op0=ALU.mult, op1=ALU.mult), [lfb], [lmb])
                        S.op("pe", lambda: nc.tensor.matmul(pm[:, 0:128], lhsT=self.ustr_f[:], rhs=Lm[:], start=True, stop=True),
                             [lmb], [pmb])
                        S.op("act", lambda: nc.scalar.activation(out=Wt[:], in_=pm[:, 0:128], func=AF.Exp,
                                                                 bias=G1[:, ti, h:h + 1], scale=1.0), [pmb, g1b], [wtb])
                        S.op("pool", lambda: nc.gpsimd.tensor_tensor(out=Wt[:], in0=Wt[:], in1=self.mscale[:], op=ALU.mult), [wtb], [wtb])
                        S.op("dve", lambda: nc.vector.tensor_tensor(out=Sw[:], in0=pS[:, 0:128], in1=Wt[:], op=ALU.mult),
                             [pSb, wtb], [swb])
                    else:
                        S.op("dve", lambda: nc.vector.tensor_tensor(out=Sw[:], in0=pS[:, 0:128], in1=self.dmaskT[:, h, :], op=ALU.mult),
                             [pSb], [swb])
                    S.op("pe", lambda: nc.tensor.matmul(pA[:, 0:512], lhsT=Sw[:], rhs=V[:, ti, :], start=True, stop=True),
                         [swb, vb[ti]], [pAb])
                    S.op("act", lambda: nc.scalar.activation(out=numA[p3][:], in_=pA[:, 0:512], func=AF.Copy), [pAb], [nab[p3]])

                    def fn():
                        nc.tensor.transpose(pK[:, 0:128], KT_[:, 0, tok], self.ident[:])
                        return nc.tensor.transpose(pK[:, 128:256], KT_[:, 1, tok], self.ident[:])
                    S.op("pe", fn, [qb_], [pKb])
                    wcol = WK[:, ti, h:h + 1] if ml else self.statew[:, h:h + 1]
                    S.op("dve", lambda: nc.vector.tensor_scalar(out=kw[:], in0=pK[:, 0:256], scalar1=wcol, scalar2=None, op0=ALU.mult),
                         [pKb] + ([gtb] if ml else []), [kwb])

                    def fn():
                        nc.tensor.matmul(pU[0][:, 0:512], lhsT=kw[:, 0:128], rhs=V[:, ti, :], start=True, stop=True)
                        return nc.tensor.matmul(pU[1][:, 0:512], lhsT=kw[:, 128:256], rhs=V[:, ti, :], start=True, stop=True)
                    S.op("pe", fn, [kwb, vb[ti]], [pUb])
                    S.op("act", lambda: nc.scalar.activation(out=Ucp[p][:, 0, :], in_=pU[0][:, 0:512], func=AF.Copy), [pUb], [ucb[p]])
                    S.op("dve", lambda: nc.vector.tensor_copy(out=Ucp[p][:, 1, :], in_=pU[1][:, 0:512]), [pUb], [ucb[p]])
                    if ml:
                        def fn():
                            nc.tensor.matmul(pm[:, 128:129], lhsT=Sw[:], rhs=self.ones_bf[:, 0:1], start=True, stop=True)
                            nc.tensor.matmul(pm[:, 132:133], lhsT=kw[:, 0:128], rhs=self.ones_bf[:, 0:1], start=True, stop=True)
                            return nc.tensor.matmul(pm[:, 133:134], lhsT=kw[:, 128:256], rhs=self.ones_bf[:, 0:1], start=True, stop=True)
                        S.op("pe", fn, [swb, kwb], [pmb])
                        S.op("act", lambda: nc.scalar.activation(out=dsb[p3][:, 0:6], in_=pm[:, 128:134], func=AF.Copy), [pmb], [dsbb[p3]])

                def part_M(ti):
                    p = ti % 2
                    p3 = ti % 3
                    tok = slice(ti * 128, (ti + 1) * 128)
                    qb_ = qkb[ti // 4]

                    def fn():
                        nc.tensor.matmul(pB[:, 0:512], lhsT=QT[:, 0, tok], rhs=Cbf[:, 0, :], start=True, stop=False)
                        return nc.tensor.matmul(pB[:, 0:512], lhsT=QT[:, 1, tok], rhs=Cbf[:, 1, :], start=False, stop=True)
                    S.op("pe", fn, [qb_, cbfb], [pBb])
                    S.op("act", lambda: nc.scalar.activation(out=numB[p3][:], in_=pB[:, 0:512], func=AF.Copy), [pBb], [nbb[p3]])
                    if ml:
                        def fn():
                            nc.tensor.matmul(pm[:, 136:137], lhsT=QT[:, 0, tok], rhs=nbf[:, 0:1], start=True, stop=False)
                            return nc.tensor.matmul(pm[:, 136:137], lhsT=QT[:, 1, tok], rhs=nbf[:, 1:2], start=False, stop=True)
                        S.op("pe", fn, [qb_, nbfb], [pmb])
                        S.op("act", lambda: nc.scalar.activation(out=dsb[p3][:, 6:7], in_=pm[:, 136:137], func=AF.Copy), [pmb], [dsbb[p3]])
                    for jj in range(2):
                        dec = EG[:, ti, h:h + 1] if ml else float(self.gamma_L[h])
                        S.op("dve", lambda: nc.vector.scalar_tensor_tensor(out=Cf[:, jj, :], in0=Cf[:, jj, :], scalar=dec,
                                                                           in1=Ucp[p][:, jj, :], op0=ALU.mult, op1=ALU.add),
                             [self.cstb, ucb[p]] + ([gtb] if ml else []), [self.cstb])
                    S.op("act", lambda: nc.scalar.activation(out=Cbf[:], in_=Cf, func=AF.Copy), [self.cstb], [cbfb])
                    if ml:
                        S.op("dve", lambda: nc.vector.scalar_tensor_tensor(out=self.nst[:, h * 2:h * 2 + 2], in0=self.nst[:, h * 2:h * 2 + 2],
                                                                           scalar=EG[:, ti, h:h + 1], in1=dsb[p3][:, 4:6],
                                                                           op0=ALU.mult, op1=ALU.add), [self.nstb, dsbb[p3], gtb], [self.nstb])
                        S.op("act", lambda: nc.scalar.activation(out=nbf[:], in_=self.nst[:, h * 2:h * 2 + 2], func=AF.Copy),
                             [self.nstb], [nbfb])

                def part_O(ti):
                    p = ti % 3
                    dcol = EB[:, ti, h:h + 1] if ml else self.idec[:, h:h + 1]
                    if ml:
                        S.op("dve", lambda: nc.vector.scalar_tensor_tensor(out=num[:], in0=numB[p][:], scalar=dcol, in1=numA[p][:],
                                                                           op0=ALU.mult, op1=ALU.add), [nbb[p], nab[p], gtb], [numb])
                        S.op("dve", lambda: nc.vector.scalar_tensor_tensor(out=sm[:, 0:1], in0=dsb[p][:, 6:7], scalar=dcol, in1=dsb[p][:, 0:1],
                                                                           op0=ALU.mult, op1=ALU.add), [dsbb[p], gtb], [smb])
                        S.op("act", lambda: nc.scalar.activation(out=junk[:], in_=num[:], func=AF.Square, accum_out=sm[:, 3:4]),
                             [numb], [junkb, smb])
                        S.op("dve", lambda: nc.vector.tensor_tensor(out=sm[:, 1:2], in0=sm[:, 0:1], in1=sm[:, 0:1], op=ALU.mult), [smb], [smb])
                        S.op("dve", lambda: nc.vector.tensor_scalar(out=sm[:, 2:3], in0=sm[:, 1:2], scalar1=1.0, scalar2=EPS,
                                                                    op0=ALU.max, op1=ALU.mult), [smb], [smb])
                        S.op("dve", lambda: nc.vector.scalar_tensor_tensor(out=sm[:, 5:6], in0=sm[:, 3:4], scalar=1.0 / 512, in1=sm[:, 2:3],
                                                                           op0=ALU.mult, op1=ALU.add), [smb], [smb])
                        S.op("act", lambda: nc.scalar.activation(out=sm[:, 5:6], in_=sm[:, 5:6], func=AF.Ln), [smb], [smb])
                        S.op("act", lambda: nc.scalar.activation(out=sm[:, 7:8], in_=sm[:, 5:6], func=AF.Exp, scale=-0.5), [smb], [smb])
                        S.op("dve", lambda: nc.vector.scalar_tensor_tensor(out=hnt[:], in0=num[:], scalar=sm[:, 7:8], in1=gsl,
                                                                           op0=ALU.mult, op1=ALU.mult), [numb, smb, hngb], [hntb])
                    else:
                        S.op("dve", lambda: nc.vector.scalar_tensor_tensor(out=num[:], in0=numB[p][:], scalar=dcol, in1=numA[p][:],
                                                                           op0=ALU.mult, op1=ALU.add, accum_out=sm[:, 0:1]),
                             [nbb[p], nab[p]], [numb, smb])
                        S.op("act", lambda: nc.scalar.activation(out=junk[:], in_=num[:], func=AF.Square, accum_out=sm[:, 1:2]),
                             [numb], [junkb, smb])
                        S.op("dve", lambda: nc.vector.tensor_scalar(out=sm[:, 2:3], in0=sm[:, 0:1], scalar1=1.0 / 512, scalar2=None, op0=ALU.mult), [smb], [smb])
                        S.op("dve", lambda: nc.vector.scalar_tensor_tensor(out=sm[:, 3:4], in0=sm[:, 2:3], scalar=-1.0, in1=sm[:, 2:3],
                                                                           op0=ALU.mult, op1=ALU.mult), [smb], [smb])
                        S.op("dve", lambda: nc.vector.scalar_tensor_tensor(out=sm[:, 4:5], in0=sm[:, 1:2], scalar=1.0 / 512, in1=sm[:, 3:4],
                                                                           op0=ALU.mult, op1=ALU.add), [smb], [smb])
                        S.op("dve", lambda: nc.vector.tensor_scalar(out=sm[:, 4:5], in0=sm[:, 4:5], scalar1=EPS, scalar2=None, op0=ALU.add), [smb], [smb])
                        S.op("act", lambda: nc.scalar.activation(out=sm[:, 4:5], in_=sm[:, 4:5], func=AF.Ln), [smb], [smb])
                        S.op("act", lambda: nc.scalar.activation(out=sm[:, 5:6], in_=sm[:, 4:5], func=AF.Exp, scale=-0.5), [smb], [smb])
                        S.op("dve", lambda: nc.vector.scalar_tensor_tensor(out=sm[:, 6:7], in0=sm[:, 2:3], scalar=-1.0, in1=sm[:, 5:6],
                                                                           op0=ALU.mult, op1=ALU.mult), [smb], [smb])
                        S.op("act", lambda: nc.scalar.activation(out=num[:], in_=num[:], func=AF.Identity, bias=sm[:, 6:7], scale=sm[:, 5:6]),
                             [numb, smb], [numb])
                        S.op("dve", lambda: nc.vector.tensor_tensor(out=hnt[:], in0=num[:], in1=gsl, op=ALU.mult), [numb, hngb], [hntb])
                    S.op("pool", lambda: nc.gpsimd.tensor_tensor(out=ho[:], in0=hnt[:], in1=OG[:, ti, :], op=ALU.mult), [hntb, ob[ti]], [hob])
                    self.out_transpose_store(ho, hob, 4 * h, ti * 128, hts, htb)

                part_F(0)
                if NT > 1:
                    part_F(1)
                part_M(0)
                for ti in range(NT):
                    if ti + 2 < NT:
                        part_F(ti + 2)
                    if ti + 1 < NT:
                        part_M(ti + 1)
                    part_O(ti)
                S.dma("sp", self.CstD[h, :, :], self.Cst[:], [self.cstb], [self.cstdb])
            S.barrier()

    def swa_stage(self, st, j):
        nc, S, TS, NT = self.nc, self.S, self.TS, self.NT
        W = self.s_w_qkv[j]
        with ExitStack() as es:
            QT = self.sb(es, "sQT", [128, 4, TS], BF16)
            KDa = self.sb(es, "sKDa", [128, 128 + TS], BF16)
            KDb = self.sb(es, "sKDb", [128, 128 + TS], BF16)
            VV = self.sb(es, "sVV", [128, NT + 1, 256], BF16)
            qb = [Buf("q") for _ in range(self.NB)]
            kdb = Buf("kd")
            vvb = [Buf("vv") for _ in range(NT + 1)]
            smx = self.sb(es, "ssm", [128, 256], F32); smxb = Buf("smx")
            pr = self.sb(es, "spr", [128, 256], BF16); prb = Buf("pr")
            pT = self.sb(es, "spT", [128, 2, 128], BF16); pTb = Buf("pT")
            sm = self.sb(es, "ssmall", [128, 8], F32); smb = Buf("sm")
            Ot = self.sb(es, "sOt", [128, 512], BF16); otb = Buf("ot")
            hts = self.sb(es, "shts", [128, 512], BF16); htb = Buf("hts")
            otbs = [Buf("ot0"), Buf("ot1"), Buf("ot2"), Buf("ot3")]
            lanes = []
            pkb = [self.pT[0], (self.psl(es, "ptl1", BF16, 1024), Buf("pK1")), (self.psl(es, "ptl2", BF16, 1024), Buf("pK2")), self.pT[1]]
            for ln in range(4):
                lanes.append({
                    "pS": self.lbank[ln],
                    "pA": self.lbank[ln],
                    "pK": pkb[ln],
                    "ko": 0,
                    "smx": (smx, smxb) if ln == 0 else (self.sb(es, "ssm1", [128, 256], F32), Buf("smx1")),
                    "pr": (pr, prb) if ln == 0 else (self.sb(es, "spr1", [128, 256], BF16), Buf("pr1")),
                    "pT": (pT, pTb) if ln == 0 else (self.sb(es, "spT1", [128, 2, 128], BF16), Buf("pT1")),
                    "sm": (sm, smb) if ln == 0 else (self.sb(es, "ssmall1", [128, 8], F32), Buf("sm1")),
                })
            S.op("act", lambda: nc.scalar.activation(out=VV[:, 0, :], in_=self.Vprev[:], func=AF.Copy), [self.vprevb], [vvb[0]])

            def v_ev(ti, pa, pb):
                self.evac_copy(VV[:, ti + 1, :], pa[:, 0:256], [pb], [vvb[ti + 1]])
            self.lin_tok(W, 0, 16, [(2304, 256)], v_ev)
            S.op("act", lambda: nc.scalar.activation(out=self.Vprev[:], in_=VV[:, NT, :], func=AF.Copy), [vvb[NT]], [self.vprevb])
            for g in range(4):
                def q_ev(gi, mi, b, pa, pb):
                    self.evac_copy(QT[:, mi, b * 512:(b + 1) * 512], pa[:, 0:512], [pb], [qb[b]])
                self.lin_feat(W, 0, 16, [[(g * 512, 512)]], self.rhs_blk, self.rhs_blk_bufs, q_ev)
                if g == 0:
                    S.op("pool", lambda: nc.gpsimd.memset(KDa[64:128, :], 0.0), [], [kdb])
                    S.op("pool", lambda: nc.gpsimd.memset(KDb[0:64, :], 0.0), [], [kdb])
                S.op("act", lambda: nc.scalar.activation(out=KDa[0:64, 0:128], in_=self.KTprev[0:64, g, :], func=AF.Copy), [self.kprevb], [kdb])
                S.op("act", lambda: nc.scalar.activation(out=KDb[64:128, 0:128], in_=self.KTprev[64:128, g, :], func=AF.Copy), [self.kprevb], [kdb])

                def k_ev(gi, mi, b, pa, pb):
                    S.op("act", lambda: nc.scalar.activation(out=KDa[0:64, 128 + b * 512:128 + (b + 1) * 512], in_=pa[0:64, 0:512], func=AF.Copy), [pb], [kdb])
                    S.op("dve", lambda: nc.vector.tensor_copy(out=KDb[64:128, 128 + b * 512:128 + (b + 1) * 512], in_=pa[64:128, 0:512]), [pb], [kdb])
                self.lin_feat(W, 0, 16, [[(2048 + g * 64, 64), (2048 + g * 64, 64)]], self.rhs_blk, self.rhs_blk_bufs, k_ev)
                S.op("act", lambda: nc.scalar.activation(out=self.KTprev[0:64, g, :], in_=KDa[0:64, TS:TS + 128], func=AF.Copy), [kdb], [self.kprevb])
                S.op("act", lambda: nc.scalar.activation(out=self.KTprev[64:128, g, :], in_=KDb[64:128, TS:TS + 128], func=AF.Copy), [kdb], [self.kprevb])
                for n in range(NT):
                    first = (st == 0 and n == 0)
                    mask = self.swa_mask[:, 0, :] if first else self.swa_mask[:, 1, :]

                    def chain(hh, lane):
                        head = 8 * g + hh
                        mi, half = hh // 2, hh % 2
                        KDh = KDa if half == 0 else KDb
                        pS, pSb = lanes[lane]["pS"]
                        pK, pKb = lanes[lane]["pK"]
                        ko = lanes[lane]["ko"]
                        pA, pAb = lanes[lane]["pA"]
                        smx_, smxb_ = lanes[lane]["smx"]
                        pr_, prb_ = lanes[lane]["pr"]
                        pT_, pTb_ = lanes[lane]["pT"]
                        sm_, smb_ = lanes[lane]["sm"]
                        S.op("pe", lambda: nc.tensor.matmul(pS[:, 0:256], lhsT=QT[:, mi, n * 128:(n + 1) * 128],
                                                            rhs=KDh[:, n * 128:n * 128 + 256], start=True, stop=True),
                             [qb[n // 4], kdb], [pSb])
                        yield
                        S.op("dve", lambda: nc.vector.scalar_tensor_tensor(out=smx_[:], in0=pS[:, 0:256], scalar=0.125, in1=mask,
                                                                           op0=ALU.mult, op1=ALU.add), [pSb], [smxb_])
                        yield
                        S.op("dve", lambda: nc.vector.reduce_max(out=sm_[:, 0:1], in_=smx_[:], axis=AX.X), [smxb_], [smb_])
                        yield
                        S.op("dve", lambda: nc.vector.tensor_scalar(out=sm_[:, 1:2], in0=sm_[:, 0:1], scalar1=self.sinks[:, head:head + 1],
                                                                    scalar2=-1.0, op0=ALU.max, op1=ALU.mult), [smb_], [smb_])
                        yield
                        S.op("act", lambda: nc.scalar.activation(out=pr_[:], in_=smx_[:], func=AF.Exp, bias=sm_[:, 1:2], scale=1.0,
                                                                 accum_out=sm_[:, 2:3]), [smxb_, smb_], [prb_, smb_])
                        yield
                        S.op("act", lambda: nc.scalar.activation(out=sm_[:, 3:4], in_=self.sinks[:, head:head + 1], func=AF.Exp,
                                                                 bias=sm_[:, 1:2], scale=1.0), [smb_], [smb_])

                        def fn():
                            nc.tensor.transpose(pK[:, ko:ko + 128], pr_[:, 0:128], self.ident[:])
                            return nc.tensor.transpose(pK[:, ko + 128:ko + 256], pr_[:, 128:256], self.ident[:])
                        S.op("pe", fn, [prb_], [pKb])
                        yield
                        S.op("dve", lambda: nc.vector.tensor_tensor(out=sm_[:, 4:5], in0=sm_[:, 2:3], in1=sm_[:, 3:4], op=ALU.add), [smb_], [smb_])
                        self.evac_copy(pT_[:].rearrange("p a b -> p (a b)"), pK[:, ko:ko + 256], [pKb], [pTb_])
                        yield
                        S.op("dve", lambda: nc.vector.reciprocal(out=sm_[:, 5:6], in_=sm_[:, 4:5]), [smb_], [smb_])

                        def fn():
                            nc.tensor.matmul(pA[:, 256:320], lhsT=pT_[:, 0, :], rhs=VV[:, n, g * 64:(g + 1) * 64], start=True, stop=False)
                            return nc.tensor.matmul(pA[:, 256:320], lhsT=pT_[:, 1, :], rhs=VV[:, n + 1, g * 64:(g + 1) * 64], start=False, stop=True)
                        S.op("pe", fn, [pTb_, vvb[n], vvb[n + 1]], [pAb])
                        yield
                        S.op("dve", lambda: nc.vector.tensor_scalar(out=Ot[:, hh * 64:(hh + 1) * 64], in0=pA[:, 256:320], scalar1=sm_[:, 5:6],
                                                                    scalar2=None, op0=ALU.mult), [pAb, smb_], [otbs[hh % 4]])
                        yield

                    for hp in range(2):
                        alive = [chain(4 * hp + ln, ln) for ln in range(4)]
                        while alive:
                            for gen in list(alive):
                                try:
                                    next(gen)
                                except StopIteration:
                                    alive.remove(gen)
                    self.out_transpose_store_multi(Ot, otbs, 4 * g, n * 128, hts, htb)
            S.barrier()

    def build(self):
        nc, T, TS = self.nc, self.T, self.TS
        es = self.es
        S = self.S = Sched(nc, es)
        self.xT = self.dram("xT", [16, 128, T])
        self.HT = self.dram("outT", [16, 128, T], kind="ExternalOutput")
        self.gains_d = self.dram("gains", [128, 256])
        self.w_up = self.dram("w_up", [4, D, 2 * DFF])
        self.cw_d = self.dram("conv_w", [128, 4 * 3 * 88])
        self.cb_d = self.dram("conv_b", [128, 4 * 88])
        self.w_down = self.dram("w_down", [4, DFF, D])
        self.m_w_in = self.dram("m_w_in", [2, D, 6152])
        self.m_gb_d = self.dram("m_gb", [128, 16])
        self.m_hn = self.dram("m_hn", [2, 128, 2048])
        self.m_w_out = self.dram("m_w_out", [2, D, D])
        self.s_w_qkv = self.dram("s_w_qkv", [1, D, 2560])
        self.sinks_d = self.dram("s_sinks", [128, 32])
        self.s_w_out = self.dram("s_w_out", [1, D, D])
        self.r_w_in = self.dram("r_w_in", [1, D, 12288])
        self.r_hn = self.dram("r_hn", [1, 128, 4096])
        self.r_w_out = self.dram("r_w_out", [1, 4096, D])
        cst = {n: self.dram(n, shp) for n, shp in [("c_ident", [128, 128]), ("c_ones", [128, 128]), ("c_tri", [128, 128]),
                                                    ("c_ustr", [128, 128]), ("c_mscale", [128, 128]), ("c_swamask", [128, 512]),
                                                    ("c_dmaskT", [128, 8 * 128]), ("c_idec", [128, 8]), ("c_statew", [128, 8])]}
        self.cosT_d = self.dram("c_cosT", [128, T])
        self.sinT_d = self.dram("c_sinT", [128, T])
        self.Y = [self.dram(f"Y{p}", [16, 128, TS], kind="Internal") for p in range(3)]
        self.CstD = self.dram("CstD", [8, 128, 1024], kind="Internal")
        self.HTmix = self.dram("HTmix", [32, 128, TS], BF16, kind="Internal")
        self.GT = self.dram("GT", [NFT, 128, TS], BF16, kind="Internal")
        self.ybuf = [Buf("Y0"), Buf("Y1"), Buf("Y2")]
        self.cstdb = Buf("CstD")
        self.htbuf = Buf("HT")
        self.srcbuf = Buf("src")
        lg = np.log(1.0 - np.power(2.0, -5.0 - np.arange(8, dtype=np.float64)))
        self.gamma_L = np.exp(lg * 128.0)
        sb = lambda n, s, d: self.sb(es, n, s, d)
        self.ident = sb("ident", [128, 128], BF16)
        self.ones_bf = sb("ones_bf", [128, 128], BF16)
        self.ones_f = sb("ones_f", [128, 128], F32)
        self.tri_f = sb("tri_f", [128, 128], F32)
        self.ustr_f = sb("ustr_f", [128, 128], F32)
        self.mscale = sb("mscale", [128, 128], F32)
        self.swa_mask = sb("swa_mask", [128, 2, 256], F32)
        self.dmaskT = sb("dmaskT", [128, 8, 128], F32)
        self.idec = sb("idec", [128, 8], F32)
        self.statew = sb("statew", [128, 8], F32)
        self.gains = sb("gains_sb", [128, 256], F32)
        self.cw = sb("cw_sb", [128, 4 * 3 * 88], F32)
        self.cbias = sb("cb_sb", [128, 4 * 88], F32)
        self.m_gb = sb("m_gb_sb", [128, 16], F32)
        self.sinks = sb("sinks_sb", [128, 32], F32)
        self.rhs3 = sb("rhsbuf", [128, 16, TS], BF16)
        self.rb = [Buf(f"rhs{b}") for b in range(TS // 256)]
        self.wslots = [sb(f"wslot{i}", [128, 16, 512], BF16) for i in range(2)]
        self.wbuf = [[Buf("w0a"), Buf("w0b")], [Buf("w1a"), Buf("w1b")]]
        self.pref = None
        self.wnext = 0
        self.lnext = 0
        self.Cst = sb("Cst", [128, 1024], F32)
        self.cstb = Buf("Cst")
        self.nst = sb("nst", [128, 8], F32)
        self.nstb = Buf("nst")
        self.KTprev = sb("KTprev", [128, 4, 128], BF16)
        self.kprevb = Buf("kprev")
        self.Vprev = sb("Vprev", [128, 256], BF16)
        self.vprevb = Buf("vprev")
        self.halo = sb("halo", [128, 2, NFT, 2], F32)
        self.halob = Buf("halo")
        ps = lambda n, dt, w: es.enter_context(nc.psum_tensor(n, [128, w], dt))
        self.lbank = [(ps(f"pl{i}", F32, 512), Buf(f"pl{i}")) for i in range(4)]
        self.pS = self.lbank[0]
        self.pA = self.lbank[1]
        self.pB = self.lbank[2]
        self.pmisc = self.lbank[3]
        self.pT = [(ps("pt0", BF16, 1024), Buf("pt0")), (ps("pt1", BF16, 1024), Buf("pt1"))]
        cb = Buf("consts")
        for dst, src in [(self.ident, "c_ident"), (self.ones_bf, "c_ones")]:
            S.dma("pool", dst[:], cst[src][:, :], [], [cb])
        for dst, src in [(self.ones_f, "c_ones"), (self.tri_f, "c_tri"), (self.ustr_f, "c_ustr"), (self.mscale, "c_mscale"),
                         (self.idec, "c_idec"), (self.statew, "c_statew")]:
            S.dma("sp", dst[:], cst[src][:, :], [], [cb])
        S.dma("sp", self.swa_mask[:].rearrange("p a b -> p (a b)"), cst["c_swamask"][:, :], [], [cb])
        S.dma("sp", self.dmaskT[:].rearrange("p a b -> p (a b)"), cst["c_dmaskT"][:, :], [], [cb])
        S.dma("sp", self.gains[:], self.gains_d[:, :], [], [cb])
        S.dma("sp", self.cw[:], self.cw_d[:, :], [], [cb])
        S.dma("sp", self.cbias[:], self.cb_d[:, :], [], [cb])
        S.dma("sp", self.m_gb[:], self.m_gb_d[:, :], [], [cb])
        S.dma("sp", self.sinks[:], self.sinks_d[:, :], [], [cb])
        for k in range(16):
            S.dma("sp", self.HT[k, :, :], self.xT[k, :, :], [], [self.htbuf])
        S.barrier()
        for l in self.layers:
            kind, j = l % 3, l // 3
            S.op("pool", lambda: nc.gpsimd.memset(self.Cst[:], 0.0), [], [self.cstb])
            for hh in range(8):
                S.dma("sp", self.CstD[hh, :, :], self.Cst[:], [self.cstb], [self.cstdb])
            S.op("pool", lambda: nc.gpsimd.memset(self.nst[:], 0.0), [], [self.nstb])
            S.op("pool", lambda: nc.gpsimd.memset(self.KTprev[:], 0.0), [], [self.kprevb])
            S.op("pool", lambda: nc.gpsimd.memset(self.Vprev[:], 0.0), [], [self.vprevb])
            S.op("pool", lambda: nc.gpsimd.memset(self.halo[:], 0.0), [], [self.halob])
            import os as _os
            mx = int(_os.environ.get("MAXSTAGE", "1000"))
            for st in range(self.NST):
                pf = None
                if kind == 1:
                    pf = (self.s_w_qkv[j], 0, 16, [(2304, 256)])
                elif kind == 2:
                    pf = (self.r_w_in[j], 0, 16, [(0, 256), (2048, 256)])
                if self.stg < mx: self.norm_stage(st, (l * 4 + 0) * 16, pf)
                self.stg += 1
                if kind == 1:
                    if self.stg < mx: self.swa_stage(st, j)
                    Wo, Kt = self.s_w_out[j], 16
                else:
                    if self.stg < mx: self.linattn_stage(st, kind, j)
                    Wo, Kt = (self.m_w_out[j], 16) if kind == 0 else (self.r_w_out[j], 32)
                self.stg += 1
                if self.stg < mx: parts = self.proj_out_stage(Wo, Kt, self.HTmix)
                self.stg += 1
                if self.stg < mx: self.finalize_stage(st, parts, (l * 4 + 1) * 16)
                self.stg += 1
                if self.stg < mx: self.norm_stage(st, (l * 4 + 2) * 16, (self.w_up[l], 0, 16, [(0, 256), (DFF, 256)]))
                self.stg += 1
                if self.stg < mx: self.ffn_up_stage(st, l)
                self.stg += 1
                if self.stg < mx: parts = self.proj_out_stage(self.w_down[l], NFT, self.GT)
                self.stg += 1
                if self.stg < mx: self.finalize_stage(st, parts, (l * 4 + 3) * 16)
                self.stg += 1
        S.barrier()
        self.es.close()
        return nc


def host_consts(T):
    c = {}
    i = np.arange(128)
    c["c_ident"] = np.eye(128, dtype=np.float32)
    c["c_ones"] = np.ones((128, 128), np.float32)
    c["c_tri"] = (i[:, None] <= i[None, :]).astype(np.float32)
    c["c_ustr"] = (i[:, None] > i[None, :]).astype(np.float32)
    c["c_mscale"] = (i[:, None] <= i[None, :]).astype(np.float32) * np.float32(256 ** -0.5)
    q = i[:, None]
    jj = np.arange(256)[None, :]
    valid = (jj > q) & (jj <= q + 128)
    m1 = np.where(valid, 0.0, -30000.0).astype(np.float32)
    m0 = np.where(valid & (jj >= 128), 0.0, -30000.0).astype(np.float32)
    c["c_swamask"] = np.concatenate([m0, m1], axis=1)
    lg = np.log(1.0 - np.power(2.0, -5.0 - np.arange(8, dtype=np.float64)))
    rel = (i[None, :] - i[:, None]).astype(np.float64)
    dm = np.where(rel[None] >= 0, np.exp(lg[:, None, None] * np.maximum(rel[None], 0.0)), 0.0) * (256 ** -0.5)
    c["c_dmaskT"] = np.ascontiguousarray(dm.transpose(1, 0, 2).reshape(128, 8 * 128)).astype(np.float32)
    c["c_idec"] = np.exp(lg[None, :] * (i[:, None] + 1.0)).astype(np.float32)
    c["c_statew"] = (np.exp(lg[None, :] * (127.0 - i[:, None])) * (256 ** -0.5)).astype(np.float32)
    half = 128
    inv = np.power(np.float32(10000.0), -np.arange(half, dtype=np.float32) / np.float32(half)).astype(np.float32)
    pos = np.arange(T, dtype=np.float32)
    ang = (pos[None, :] * inv[:, None]).astype(np.float32)
    c["c_cosT"] = np.cos(ang).astype(np.float32)
    c["c_sinT"] = np.sin(ang).astype(np.float32)
    return c


def host_layout(inp):
    o = {}
    g = inp["norm_gains"].reshape(4, 4, 16, 128)
    o["gains"] = np.ascontiguousarray(g.transpose(3, 0, 1, 2).reshape(128, 256))
    cw = inp["ffn_conv_w"].reshape(4, 3, 88, 128)
    o["conv_w"] = np.ascontiguousarray(cw.transpose(3, 0, 1, 2).reshape(128, 4 * 3 * 88))
    cbv = inp["ffn_conv_b"].reshape(4, 88, 128)
    o["conv_b"] = np.ascontiguousarray(cbv.transpose(2, 0, 1).reshape(128, 4 * 88))
    o["w_up"] = inp["ffn_w_up"]
    o["w_down"] = inp["ffn_w_down"]
    o["m_w_in"] = inp["mlstm_w_in"]
    gb = inp["mlstm_gate_b"].reshape(2, 8)
    o["m_gb"] = np.ascontiguousarray(np.broadcast_to(gb.reshape(1, 16), (128, 16)))
    o["m_hn"] = np.ascontiguousarray(np.broadcast_to(inp["mlstm_head_norm"][:, None, :], (2, 128, 2048)))
    o["m_w_out"] = inp["mlstm_w_out"]
    o["s_w_qkv"] = inp["swa_w_qkv"]
    o["s_sinks"] = np.ascontiguousarray(np.broadcast_to(inp["swa_sinks"].reshape(1, 32), (128, 32)))
    o["s_w_out"] = inp["swa_w_out"]
    o["r_w_in"] = inp["ret_w_in"]
    o["r_hn"] = np.ascontiguousarray(np.broadcast_to(inp["ret_head_norm"][:, None, :], (1, 128, 4096)))
    o["r_w_out"] = inp["ret_w_out"]
    return {k: np.ascontiguousarray(v, dtype=np.float32) for k, v in o.items()}


def run(inp, T, TS, layers, ncores):
    x = inp["x"]
    b = Builder(T, TS, layers)
    nc = b.build()
    shared = host_layout(inp)
    shared.update(host_consts(T))
    in_maps = []
    for c in range(ncores):
        m = dict(shared)
        xc = x[c]
        m["xT"] = np.ascontiguousarray(xc.T.reshape(16, 128, T))
        in_maps.append(m)
    res = run_bass_kernel_spmd(nc, in_maps, core_ids=list(range(ncores)))
    outs = []
    for c in range(ncores):
        oT = res.results[c]["outT"]
        outs.append(np.ascontiguousarray(oT.reshape(2048, T).T))
    return np.stack(outs, axis=0).astype(np.float32)


def kernel(**inputs):
    inp = {k: np.asarray(v) for k, v in inputs.items()}
    return run(inp, 4096, 2048, [0, 1, 2, 3], 4)
```
